# Optimizing a Trainium2 kernel written in Bass

```python
import math
import jax, jax.numpy as jnp
from jax import lax
import numpy as np

D_MODEL = 1024
BATCH = 16
SEQ = 2048
DEPTH = 2

MIX_WIDTH = 384
N_BRANCH = 3
S5_GROUP = 16
S5_GROUPS = MIX_WIDTH // S5_GROUP
S5_STATE = 64
S5_DT_MIN = 1e-3
S5_DT_MAX = 1e-1
SB_HEAD_DIM = 64
SB_HEADS = MIX_WIDTH // SB_HEAD_DIM
Q_BLOCK = 128
LRU_BLOCKS = 6
LRU_BLOCK = MIX_WIDTH // LRU_BLOCKS
CONV_WIDTH = 4
LRU_C = 8.0
IN_WIDTH = 6 * MIX_WIDTH + N_BRANCH * D_MODEL
SPLITS = [MIX_WIDTH * i for i in range(1, 7)]
D_FF = 2816
N_EXPERTS = 8
TOP_K = 2
D_FF_EXPERT = 3584
N_DENSE = (DEPTH + 1) // 2
N_MOE = DEPTH // 2
RMS_EPS = 1e-6

kernel_name = 'hybrid_s5_stickbreak_rglru_moe'


def rms_norm(x, g):
    xf = x.astype(jnp.float32)
    y = xf * lax.rsqrt(jnp.mean(xf * xf, axis=-1, keepdims=True) + RMS_EPS)
    return (y * g.astype(jnp.float32)).astype(x.dtype)


def _cmul(ar, ai, br, bi):
    return ar * br - ai * bi, ar * bi + ai * br


def s5_mixer(u, lam_re, lam_im, log_dt, b_re, b_im, c_re, c_im, d, w_glu, b_glu):
    Bsz, T, W = u.shape
    f32 = jnp.float32
    lam_re = lam_re.astype(f32)
    lam_im = lam_im.astype(f32)
    dt = jnp.exp(log_dt.astype(f32))[:, None]
    ea = jnp.exp(lam_re * dt)
    abar_re = ea * jnp.cos(lam_im * dt)
    abar_im = ea * jnp.sin(lam_im * dt)
    den = lam_re * lam_re + lam_im * lam_im
    nr = abar_re - 1.0
    coef_re = (nr * lam_re + abar_im * lam_im) / den
    coef_im = (abar_im * lam_re - nr * lam_im) / den
    bbar_re, bbar_im = _cmul(coef_re[..., None], coef_im[..., None],
                             b_re.astype(f32), b_im.astype(f32))
    ug = u.astype(f32).reshape(Bsz, T, S5_GROUPS, S5_GROUP)
    bu_re = jnp.einsum('btgh,gph->btgp', ug, bbar_re)
    bu_im = jnp.einsum('btgh,gph->btgp', ug, bbar_im)
    a_re = jnp.broadcast_to(abar_re, bu_re.shape)
    a_im = jnp.broadcast_to(abar_im, bu_im.shape)

    def combine(e1, e2):
        a1r, a1i, b1r, b1i = e1
        a2r, a2i, b2r, b2i = e2
        ar, ai = _cmul(a2r, a2i, a1r, a1i)
        br, bi = _cmul(a2r, a2i, b1r, b1i)
        return ar, ai, br + b2r, bi + b2i

    _, _, s_re, s_im = lax.associative_scan(combine, (a_re, a_im, bu_re, bu_im), axis=1)
    y = (jnp.einsum('btgp,ghp->btgh', s_re, c_re.astype(f32))
         - jnp.einsum('btgp,ghp->btgh', s_im, c_im.astype(f32)))
    y = y.reshape(Bsz, T, W) + d.astype(f32) * u.astype(f32)
    y = jax.nn.gelu(y).astype(u.dtype)
    return y * jax.nn.sigmoid(y @ w_glu + b_glu)


def stick_breaking_attention(q, k, v):
    Bsz, T, H, Dh = q.shape
    nb = T // Q_BLOCK
    scale = Dh ** -0.5
    qb = q.reshape(Bsz, nb, Q_BLOCK, H, Dh).transpose(1, 0, 2, 3, 4)
    key_pos = jnp.arange(T)

    def block(args):
        qi, i = args
        z = jnp.einsum('bqhd,bkhd->bhqk', qi, k).astype(jnp.float32) * scale
        q_pos = i * Q_BLOCK + jnp.arange(Q_BLOCK)
        causal = key_pos[None, :] < q_pos[:, None]
        log_beta = jax.nn.log_sigmoid(z)
        log_1m = jnp.where(causal, jax.nn.log_sigmoid(-z), 0.0)
        rest = lax.cumsum(log_1m, axis=3, reverse=True) - log_1m
        w = jnp.where(causal, jnp.exp(log_beta + rest), 0.0)
        return jnp.einsum('bhqk,bkhd->bqhd', w.astype(v.dtype), v)

    out = lax.map(block, (qb, jnp.arange(nb)))
    return out.transpose(1, 0, 2, 3, 4).reshape(Bsz, T, H * Dh)


def rglru_branch(xb, yb, conv_w, conv_b, w_a, b_a, w_x, b_x, lam):
    Bsz, T, W = xb.shape
    xc = lax.conv_general_dilated(xb, conv_w[:, None, :], window_strides=(1,),
                                  padding=[(CONV_WIDTH - 1, 0)],
                                  dimension_numbers=('NWC', 'WIO', 'NWC'),
                                  feature_group_count=W) + conv_b
    xh = xc.reshape(Bsz, T, LRU_BLOCKS, LRU_BLOCK)
    r = jax.nn.sigmoid(jnp.einsum('bthi,hij->bthj', xh, w_a).reshape(Bsz, T, W) + b_a)
    ig = jax.nn.sigmoid(jnp.einsum('bthi,hij->bthj', xh, w_x).reshape(Bsz, T, W) + b_x)
    log_a = LRU_C * r.astype(jnp.float32) * jax.nn.log_sigmoid(lam.astype(jnp.float32))
    a = jnp.exp(log_a)
    b = jnp.sqrt(-jnp.expm1(2.0 * log_a)) * (ig * xc).astype(jnp.float32)

    def combine(e1, e2):
        a1, b1 = e1
        a2, b2 = e2
        return a1 * a2, a2 * b1 + b2

    _, h = lax.associative_scan(combine, (a, b), axis=1)
    return h.astype(xb.dtype) * jax.nn.gelu(yb)


def swiglu(h, w_gate, w_up, w_down):
    return (jax.nn.silu(h @ w_gate) * (h @ w_up)) @ w_down


def moe_swiglu(h, router_w, router_b, w_gate, w_up, w_down):
    Bsz, T, D = h.shape
    hf = h.reshape(-1, D)
    logits = (hf @ router_w).astype(jnp.float32) + router_b.astype(jnp.float32)
    top_vals, top_idx = lax.top_k(logits, TOP_K)
    top_w = jax.nn.softmax(top_vals, axis=-1)
    comb = jnp.sum(jax.nn.one_hot(top_idx, N_EXPERTS, dtype=jnp.float32) * top_w[..., None],
                   axis=1).astype(h.dtype)
    out = jnp.zeros_like(hf)
    for e in range(N_EXPERTS):
        out = out + comb[:, e:e + 1] * swiglu(hf, w_gate[e], w_up[e], w_down[e])
    return out.reshape(Bsz, T, D)


def setup_inputs(seed: int = 0) -> dict:
    key = jax.random.key(seed)
    ks = iter(jax.random.split(key, 40))
    f32 = jnp.float32

    def nrm(shape, scale):
        return jax.random.normal(next(ks), shape, f32) * scale

    W, G, P, H = MIX_WIDTH, S5_GROUPS, S5_STATE, S5_GROUP
    a8 = jax.random.uniform(next(ks), (DEPTH, W), f32, 0.9, 0.999)
    a_lru = a8 ** (1.0 / LRU_C)
    return {
        'x': nrm((BATCH, SEQ, D_MODEL), 1.0),
        'mix_norm_g': 1.0 + nrm((DEPTH, D_MODEL), 0.02),
        'w_in': nrm((DEPTH, D_MODEL, IN_WIDTH), D_MODEL ** -0.5),
        'gate_b': nrm((DEPTH, N_BRANCH * D_MODEL), 0.02),
        's5_lambda_re': -0.5 + nrm((DEPTH, G, P), 0.01),
        's5_lambda_im': jnp.pi * jnp.arange(P, dtype=f32) + nrm((DEPTH, G, P), 0.01),
        's5_log_dt': jax.random.uniform(next(ks), (DEPTH, G), f32,
                                        math.log(S5_DT_MIN), math.log(S5_DT_MAX)),
        's5_b_re': nrm((DEPTH, G, P, H), (2.0 * H) ** -0.5),
        's5_b_im': nrm((DEPTH, G, P, H), (2.0 * H) ** -0.5),
        's5_c_re': nrm((DEPTH, G, H, P), P ** -0.5),
        's5_c_im': nrm((DEPTH, G, H, P), P ** -0.5),
        's5_d': nrm((DEPTH, W), 1.0),
        's5_w_glu': nrm((DEPTH, W, W), W ** -0.5),
        's5_b_glu': nrm((DEPTH, W), 0.02),
        'conv_w': nrm((DEPTH, CONV_WIDTH, W), CONV_WIDTH ** -0.5),
        'conv_b': nrm((DEPTH, W), 0.02),
        'lru_w_a': nrm((DEPTH, LRU_BLOCKS, LRU_BLOCK, LRU_BLOCK), LRU_BLOCK ** -0.5),
        'lru_b_a': nrm((DEPTH, W), 0.02),
        'lru_w_x': nrm((DEPTH, LRU_BLOCKS, LRU_BLOCK, LRU_BLOCK), LRU_BLOCK ** -0.5),
        'lru_b_x': nrm((DEPTH, W), 0.02),
        'lru_lambda': jnp.log(a_lru) - jnp.log1p(-a_lru),
        'w_branch': nrm((DEPTH, N_BRANCH, W, D_MODEL), W ** -0.5),
        'w_out': nrm((DEPTH, D_MODEL, D_MODEL), D_MODEL ** -0.5),
        'ffn_norm_g': 1.0 + nrm((DEPTH, D_MODEL), 0.02),
        'ffn_w_gate': nrm((N_DENSE, D_MODEL, D_FF), D_MODEL ** -0.5),
        'ffn_w_up': nrm((N_DENSE, D_MODEL, D_FF), D_MODEL ** -0.5),
        'ffn_w_down': nrm((N_DENSE, D_FF, D_MODEL), D_FF ** -0.5),
        'router_w': nrm((N_MOE, D_MODEL, N_EXPERTS), D_MODEL ** -0.5),
        'router_b': nrm((N_MOE, N_EXPERTS), 0.01),
        'moe_w_gate': nrm((N_MOE, N_EXPERTS, D_MODEL, D_FF_EXPERT), D_MODEL ** -0.5),
        'moe_w_up': nrm((N_MOE, N_EXPERTS, D_MODEL, D_FF_EXPERT), D_MODEL ** -0.5),
        'moe_w_down': nrm((N_MOE, N_EXPERTS, D_FF_EXPERT, D_MODEL), D_FF_EXPERT ** -0.5),
        'final_norm_g': 1.0 + nrm((D_MODEL,), 0.02),
    }


def reference(x, mix_norm_g, w_in, gate_b, s5_lambda_re, s5_lambda_im, s5_log_dt,
              s5_b_re, s5_b_im, s5_c_re, s5_c_im, s5_d, s5_w_glu, s5_b_glu,
              conv_w, conv_b, lru_w_a, lru_b_a, lru_w_x, lru_b_x, lru_lambda,
              w_branch, w_out, ffn_norm_g, ffn_w_gate, ffn_w_up, ffn_w_down,
              router_w, router_b, moe_w_gate, moe_w_up, moe_w_down, final_norm_g):
    Bsz, T, _ = x.shape
    for layer in range(DEPTH):
        h = rms_norm(x, mix_norm_g[layer])
        proj = h @ w_in[layer]
        u_s5, q, k, v, x_lru, y_lru, g = jnp.split(proj, SPLITS, axis=-1)
        a_out = s5_mixer(u_s5, s5_lambda_re[layer], s5_lambda_im[layer], s5_log_dt[layer],
                         s5_b_re[layer], s5_b_im[layer], s5_c_re[layer], s5_c_im[layer],
                         s5_d[layer], s5_w_glu[layer], s5_b_glu[layer])
        hs = (Bsz, T, SB_HEADS, SB_HEAD_DIM)
        b_out = stick_breaking_attention(q.reshape(hs), k.reshape(hs), v.reshape(hs))
        c_out = rglru_branch(x_lru, y_lru, conv_w[layer], conv_b[layer], lru_w_a[layer],
                             lru_b_a[layer], lru_w_x[layer], lru_b_x[layer], lru_lambda[layer])
        branches = jnp.stack([a_out, b_out, c_out], axis=2)
        br_d = jnp.einsum('btnw,nwd->btnd', branches, w_branch[layer])
        gates = jax.nn.sigmoid(g + gate_b[layer]).reshape(Bsz, T, N_BRANCH, D_MODEL)
        merged = jnp.sum(gates * br_d, axis=2)
        x = x + merged @ w_out[layer]
        h = rms_norm(x, ffn_norm_g[layer])
        if layer % 2 == 0:
            j = layer // 2
            x = x + swiglu(h, ffn_w_gate[j], ffn_w_up[j], ffn_w_down[j])
        else:
            j = layer // 2
            x = x + moe_swiglu(h, router_w[j], router_b[j], moe_w_gate[j],
                               moe_w_up[j], moe_w_down[j])
    return rms_norm(x, final_norm_g)
```

```python
import math
import numpy as np
from contextlib import ExitStack
import concourse.bass as bass
import concourse.mybir as mybir
from concourse.bass_utils import run_bass_kernel_spmd

F32 = mybir.dt.float32
BF16 = mybir.dt.bfloat16
U8 = mybir.dt.uint8
AF = mybir.ActivationFunctionType
ALU = mybir.AluOpType

ENGS = ("pe", "act", "dve", "pool", "sp")
EPOCH = 30000
DEFER = False
PRUNE = "pe"
PG = 512

D = 1024
T = 2048
NCHUNK = 512
W = 384
INW = 5376
DFF = 2816
DFE = 3584
NE = 8
LS5 = 128
PI = math.pi


def _dsize(dt):
    return mybir.dt.size(dt)


def keys_of(a):
    if not hasattr(a, "tensor"):
        return [a]
    t = a.tensor
    es = _dsize(a.dtype)
    shp = list(t.shape)
    rb = 1
    for s in shp[1:]:
        rb *= s
    rb *= _dsize(t.dtype)
    ob = a.offset * es
    col0 = ob % rb
    ext = es
    for (step, cnt) in list(a.ap)[1:]:
        ext += (cnt - 1) * abs(step) * es
    p0 = col0 // PG
    p1 = (col0 + ext - 1) // PG
    return [(t.name, p) for p in range(p0, p1 + 1)]


class Prog:
    def __init__(self, nc):
        self.nc = nc
        self.ops = []
        self.last_w = {}
        self.readers = {}
        self.dma_cnt = {}

    def op(self, eng, fn, r=(), w=(), dma=False, sem=None, final=False):
        idx = len(self.ops)
        rk = []
        for a in r:
            rk.extend(keys_of(a))
        wk = []
        for a in w:
            wk.extend(keys_of(a))
        deps = set()
        for k in rk:
            lw = self.last_w.get(k)
            if lw is not None:
                deps.add(lw)
        for k in wk:
            lw = self.last_w.get(k)
            if lw is not None:
                deps.add(lw)
            for rd in self.readers.get(k, ()):
                deps.add(rd)
        deps.discard(idx)
        if eng == "pe":
            deps = {d for d in deps if self.ops[d]["eng"] != "pe"}
        dma_need = {}
        latest = {}
        for d_ in deps:
            od = self.ops[d_]
            if od["dma"]:
                dma_need["d_" + od["sem"]] = 16 * self.dma_cnt[od["sem"]]
            else:
                if latest.get(od["eng"], -1) < d_:
                    latest[od["eng"]] = d_
        if PRUNE == "pe":
            deps = {d_ for d_ in deps if self.ops[d_]["dma"] or self.ops[d_]["eng"] != "pe"}
            if "pe" in latest:
                deps.add(latest["pe"])
        elif PRUNE == "all":
            deps = {d_ for d_ in deps if self.ops[d_]["dma"]} | set(latest.values())
        rec = dict(eng=eng, fn=fn, deps=sorted(deps), dma=dma, sem=None, cnt=None,
                   signal=False, final=final, dma_need=dma_need)
        if dma:
            assert sem is not None
            c = self.dma_cnt.get(sem, 0) + 1
            self.dma_cnt[sem] = c
            rec["sem"] = sem
            rec["cnt"] = c
            rec["signal"] = True
        self.ops.append(rec)
        for k in rk:
            self.readers.setdefault(k, []).append(idx)
        for k in wk:
            self.last_w[k] = idx
            self.readers[k] = []
        return idx

    def emit(self, stack):
        nc = self.nc
        ops = self.ops
        for o in ops:
            for d in o["deps"]:
                ops[d]["signal"] = True
        eng_sig = {e: 0 for e in ENGS}
        for o in ops:
            if o["dma"]:
                continue
            if o["signal"]:
                eng_sig[o["eng"]] += 1
                o["cnt"] = eng_sig[o["eng"]]
        sems = {}

        def getsem(name):
            if name not in sems:
                sems[name] = stack.enter_context(nc.semaphore(name))
            return sems[name]

        for e in ENGS:
            for ep in range(eng_sig[e] // EPOCH + 1):
                getsem(f"e_{e}_{ep}")
        for name in self.dma_cnt:
            getsem("d_" + name)

        def token(o):
            if o["dma"]:
                return ("d_" + o["sem"], 16 * o["cnt"])
            c = o["cnt"]
            ep = (c - 1) // EPOCH
            return (f"e_{o['eng']}_{ep}", c - ep * EPOCH)

        streams = {e: [] for e in ENGS}
        for i, o in enumerate(ops):
            streams[o["eng"]].append(i)
        block = stack.enter_context(nc.Block())

        def run_stream(e, engobj):
            waited = {}
            for i in streams[e]:
                o = ops[i]
                need = {}
                for d in o["deps"]:
                    if ops[d]["dma"]:
                        continue
                    sn, v = token(ops[d])
                    if need.get(sn, 0) < v:
                        need[sn] = v
                for sn, v in o["dma_need"].items():
                    if need.get(sn, 0) < v:
                        need[sn] = v
                for sn, v in need.items():
                    if waited.get(sn, 0) >= v:
                        continue
                    engobj.wait_ge(sems[sn], v)
                    waited[sn] = v
                ins = o["fn"](engobj)
                if o["signal"]:
                    sn, v = token(o)
                    ins.then_inc(sems[sn], 16 if o["dma"] else 1)
            for i in streams[e]:
                o = ops[i]
                if o["dma"] and o["final"]:
                    sn, v = token(o)
                    if waited.get(sn, 0) < v:
                        engobj.wait_ge(sems[sn], v)
                        waited[sn] = v

        @block.tensor
        def _(eng):
            run_stream("pe", eng)

        @block.scalar
        def _(eng):
            run_stream("act", eng)

        @block.vector
        def _(eng):
            run_stream("dve", eng)

        @block.gpsimd
        def _(eng):
            run_stream("pool", eng)

        @block.sync
        def _(eng):
            run_stream("sp", eng)


class Arena:
    def __init__(self, nc, stack, nbytes):
        self.t = stack.enter_context(nc.sbuf_tensor("arena", [128, nbytes], U8))
        self.n = nbytes
        self.top = 0
        self.peak = 0

    def alloc(self, shape, dt=F32, align=PG):
        n = _dsize(dt)
        for s in shape:
            n *= s
        off = (self.top + align - 1) // align * align
        assert off + n <= self.n, f"arena overflow: need {off + n} have {self.n}"
        self.top = off + n
        self.peak = max(self.peak, self.top)
        v = self.t[:, off:off + n].bitcast(dt)
        if len(shape) == 2:
            v = v.rearrange("p (a b) -> p a b", a=shape[0])
        elif len(shape) == 3:
            v = v.rearrange("p (a b c) -> p a b c", a=shape[0], b=shape[1])
        return v

    def mark(self):
        return self.top

    def release(self, m):
        self.top = m


WSPEC = {}


def build(NSEQ=2, NCH=4, DEPTH=2, dbg=(), skip_conv=False):
    nc = bass.Bass("TRN2", target_bir_lowering=False)
    P = Prog(nc)
    st = ExitStack()
    dbg_out = {}

    def din(name, shape):
        return nc.dram_tensor(name, list(shape), F32, kind="ExternalInput").ap()

    x = din("x", [NSEQ, T, D])
    mix_norm_g = din("mix_norm_g", [2, D])
    w_in = din("w_in", [2, D, INW])
    gate_b = din("gate_b", [2, 3 * D])
    s5_lambda_re = din("s5_lambda_re", [2, 24, 64])
    s5_lambda_im = din("s5_lambda_im", [2, 24, 64])
    s5_log_dt = din("s5_log_dt", [2, 24])
    s5_b_re = din("s5_b_re", [2, 24, 64, 16])
    s5_b_im = din("s5_b_im", [2, 24, 64, 16])
    s5_c_re = din("s5_c_re", [2, 24, 16, 64])
    s5_c_im = din("s5_c_im", [2, 24, 16, 64])
    s5_d = din("s5_d", [2, W])
    s5_w_glu = din("s5_w_glu", [2, W, W])
    s5_b_glu = din("s5_b_glu", [2, W])
    conv_w = din("conv_w", [2, 4, W])
    conv_b = din("conv_b", [2, W])
    lru_w_a = din("lru_w_a", [2, 6, 64, 64])
    lru_b_a = din("lru_b_a", [2, W])
    lru_w_x = din("lru_w_x", [2, 6, 64, 64])
    lru_b_x = din("lru_b_x", [2, W])
    lru_lambda = din("lru_lambda", [2, W])
    w_branch = din("w_branch", [2, 3, W, D])
    w_out = din("w_out", [2, D, D])
    ffn_norm_g = din("ffn_norm_g", [2, D])
    ffn_w_gate = din("ffn_w_gate", [1, D, DFF])
    ffn_w_up = din("ffn_w_up", [1, D, DFF])
    ffn_w_down = din("ffn_w_down", [1, DFF, D])
    router_w = din("router_w", [1, D, NE])
    router_b = din("router_b", [1, NE])
    moe_w_gate = din("moe_w_gate", [1, NE, D, DFE])
    moe_w_up = din("moe_w_up", [1, NE, D, DFE])
    moe_w_down = din("moe_w_down", [1, NE, DFE, D])
    final_norm_g = din("final_norm_g", [D])
    out = nc.dram_tensor("out", [NSEQ, T, D], F32, kind="ExternalOutput").ap()

    A = Arena(nc, st, 200704)
    psb = [st.enter_context(nc.psum_tensor(f"ps{i}", [128, 512], F32)).ap() for i in range(8)]
    ps_rr = [0]

    def nb():
        i = ps_rr[0]
        ps_rr[0] = (i + 1) % 8
        return psb[i]

    def dump(name, ap2d, shape):
        if name not in dbg:
            return
        o = nc.dram_tensor("dbg_" + name, list(shape), ap2d.dtype, kind="ExternalOutput").ap()
        dbg_out[name] = o
        P.op("sp", lambda e: e.dma_start(out=o, in_=ap2d), r=[ap2d], w=["dbg_" + name],
             dma=True, sem="dbg_" + name, final=True)

    def act(out_, in_, func, r=None, **kw):
        rr = [in_] + [v for v in kw.values() if hasattr(v, "tensor")]
        P.op("act", lambda e: e.activation(out=out_, in_=in_, func=func, **kw), r=rr, w=[out_])

    def tt(eng, out_, a, b, op):
        P.op(eng, lambda e: e.tensor_tensor(out=out_, in0=a, in1=b, op=op), r=[a, b], w=[out_])

    def ts(eng, out_, a, s1, op0, s2=None, op1=None):
        rr = [a] + [s for s in (s1, s2) if hasattr(s, "tensor")]
        if op1 is None:
            P.op(eng, lambda e: e.tensor_scalar(out=out_, in0=a, scalar1=s1, scalar2=None, op0=op0), r=rr, w=[out_])
        else:
            P.op(eng, lambda e: e.tensor_scalar(out=out_, in0=a, scalar1=s1, scalar2=s2, op0=op0, op1=op1), r=rr, w=[out_])

    def stt(out_, a, s, b, op0, op1):
        rr = [a, b] + ([s] if hasattr(s, "tensor") else [])
        P.op("dve", lambda e: e.scalar_tensor_tensor(out=out_, in0=a, scalar=s, in1=b, op0=op0, op1=op1), r=rr, w=[out_])

    def cp(eng, out_, in_):
        if eng == "act":
            P.op("act", lambda e: e.activation(out=out_, in_=in_, func=AF.Copy), r=[in_], w=[out_])
        else:
            P.op(eng, lambda e: e.tensor_copy(out=out_, in_=in_), r=[in_], w=[out_])

    def memset(eng, ap, val):
        P.op(eng, lambda e: e.memset(ap, val), w=[ap])

    def mm(out_, lhsT, rhs, start, stop):
        P.op("pe", lambda e: e.matmul(out_, lhsT=lhsT, rhs=rhs, start=start, stop=stop), r=[lhsT, rhs], w=[out_])

    def tr(out_, in_, ident):
        P.op("pe", lambda e: e.transpose(out=out_, in_=in_, identity=ident), r=[in_, ident], w=[out_])

    def dma(out_, in_, sem, r=(), w=(), final=False, slow=False):
        rr = list(r) + ([in_] if in_.tensor.name == "arena" else [])
        ww = list(w) + ([out_] if out_.tensor.name == "arena" else [])
        if slow:
            P.op("sp", lambda e: e.dma_start(out=out_, in_=in_, allow_slow_non_contiguous=True), r=rr, w=ww, dma=True, sem=sem, final=final)
        else:
            P.op("sp", lambda e: e.dma_start(out=out_, in_=in_), r=rr, w=ww, dma=True, sem=sem, final=final)

    ident_f = A.alloc([128], F32)
    ones_f = A.alloc([128], F32)
    ident_b = A.alloc([128], BF16, align=256)
    ones_b = A.alloc([128], BF16, align=256)
    trineg = A.alloc([128], BF16, align=256)
    masks = A.alloc([4, 512], BF16)
    tmpc = A.alloc([512], F32)
    memset("pool", ones_f, 1.0)
    P.op("pool", lambda e: e.affine_select(out=ident_f, in_=ones_f, pattern=[[-1, 128]], compare_op=ALU.is_equal,
                                           fill=0.0, base=0, channel_multiplier=1), r=[ones_f], w=[ident_f])
    cp("pool", ident_b, ident_f)
    cp("pool", ones_b, ones_f)
    memset("pool", tmpc, -1.0)
    P.op("pool", lambda e: e.affine_select(out=tmpc[:, 0:128], in_=tmpc[:, 0:128], pattern=[[-1, 128]], compare_op=ALU.is_ge,
                                           fill=0.0, base=0, channel_multiplier=1), r=[tmpc], w=[tmpc])
    cp("pool", trineg, tmpc[:, 0:128])
    for r_ in range(4):
        memset("pool", tmpc, 1.0)
        P.op("pool", lambda e, r_=r_: e.affine_select(out=tmpc, in_=tmpc, pattern=[[1, 512]], compare_op=ALU.is_gt,
                                                     fill=0.0, base=-128 * r_, channel_multiplier=-1), r=[tmpc], w=[tmpc])
        cp("pool", masks[:, r_, :], tmpc)

    rows = {}
    nrow = [0, 0]
    stage = [A.alloc([128], F32), A.alloc([128], F32)]
    memset("dve", stage[0], 0.0)
    memset("dve", stage[1], 0.0)
    ptab = [A.alloc([128], F32), A.alloc([128], F32)]
    pcount = [0]

    def addvec(name, l, src2d, n, si):
        r0 = nrow[si]
        nrow[si] += n
        assert nrow[si] <= 128
        rows[(name, l)] = (si, r0)
        pcount[0] += 1
        dma(stage[si][r0:r0 + n, :], src2d, sem="pstage", r=["in_" + name])

    ldt_s = A.alloc([2, 2], F32, align=64)
    for l in range(2):
        nrow[l] = 12
        rows[("log_dt", l)] = (l, 0)
        dma(ldt_s[0:12, l, :], s5_log_dt[l].rearrange("(m two) -> m two", two=2), sem=f"pldt{l}")
        cp("dve", stage[l][0:12, :].rearrange("m (two q) -> m two q", two=2),
           ldt_s[0:12, l, :].unsqueeze(2).to_broadcast([12, 2, 64]))
    for l in range(2):
        addvec("mix_g", l, mix_norm_g[l].rearrange("(r c) -> r c", c=128), 8, l)
        addvec("ffn_g", l, ffn_norm_g[l].rearrange("(r c) -> r c", c=128), 8, l)
        addvec("gate_b", l, gate_b[l].rearrange("(r c) -> r c", c=128), 24, l)
        addvec("s5_d", l, s5_d[l].rearrange("(r c) -> r c", c=128), 3, l)
        addvec("b_glu", l, s5_b_glu[l].rearrange("(r c) -> r c", c=128), 3, l)
        addvec("conv_w", l, conv_w[l].rearrange("k (r c) -> (k r) c", c=128), 12, l)
        addvec("conv_b", l, conv_b[l].rearrange("(r c) -> r c", c=128), 3, l)
        addvec("b_a", l, lru_b_a[l].rearrange("(r c) -> r c", c=128), 3, l)
        addvec("b_x", l, lru_b_x[l].rearrange("(r c) -> r c", c=128), 3, l)
        addvec("lam", l, lru_lambda[l].rearrange("(r c) -> r c", c=128), 3, l)
        addvec("lam_re", l, s5_lambda_re[l].rearrange("(m two) q -> m (two q)", two=2), 12, l)
        addvec("lam_im", l, s5_lambda_im[l].rearrange("(m two) q -> m (two q)", two=2), 12, l)
    addvec("fin_g", 0, final_norm_g.rearrange("(r c) -> r c", c=128), 8, 0)
    for si in range(2):
        pb = nb()
        tr(pb[:, 0:128], stage[si], ident_f)
        cp("dve", ptab[si], pb[:, 0:128])

    def pcol(name, l, j=0, n=1):
        si, r0 = rows[(name, l)]
        return ptab[si][:, r0 + j:r0 + j + n]

    rw = A.alloc([8, 8], F32, align=64)
    rb_bc = A.alloc([8], F32, align=64)
    dma(rw, router_w[0].rearrange("(kt p) e -> p kt e", p=128), sem="pstage2")
    dma(rb_bc, router_b[0:1, :].to_broadcast([128, 8]), sem="pstage2")

    scr = {}

    def declare_w(name, K, N, cb):
        kt = K // 128
        nt = N // cb
        t = nc.dram_tensor("scr_" + name, [nt, 128, kt, cb], BF16, kind="Internal").ap()
        scr[name] = (t, kt, nt, cb)

    conv_jobs = []
    for l in range(2):
        declare_w(f"win{l}", D, INW, 384)
        conv_jobs.append((f"win{l}", w_in[l], l))
        for n in range(3):
            declare_w(f"wbr{l}_{n}", W, D, 256)
            conv_jobs.append((f"wbr{l}_{n}", w_branch[l, n], l))
        declare_w(f"wout{l}", D, D, 256)
        conv_jobs.append((f"wout{l}", w_out[l], l))
        declare_w(f"glu{l}", W, W, 384)
        conv_jobs.append((f"glu{l}", s5_w_glu[l], l))
    declare_w("fg", D, DFF, 256)
    declare_w("fu", D, DFF, 256)
    declare_w("fd", DFF, D, 128)
    conv_jobs += [("fg", ffn_w_gate[0], 0), ("fu", ffn_w_up[0], 0), ("fd", ffn_w_down[0], 0)]
    for e_ in range(NE):
        declare_w(f"mg{e_}", D, DFE, 256)
        declare_w(f"mu{e_}", D, DFE, 256)
        declare_w(f"md{e_}", DFE, D, 128)
        conv_jobs += [(f"mg{e_}", moe_w_gate[0, e_], 1), (f"mu{e_}", moe_w_up[0, e_], 1), (f"md{e_}", moe_w_down[0, e_], 1)]
    xmid = nc.dram_tensor("xmid", [NSEQ, NCH, 128, 8, NCHUNK], F32, kind="Internal").ap()

    if not skip_conv:
        mk = A.mark()
        NST = 8
        stg = [A.alloc([2048], F32) for _ in range(NST)]
        stb = [A.alloc([2048], BF16) for _ in range(NST)]
        step = 0
        cast_engs = ["dve", "act"]
        pend = []

        def finish(job):
            (i, st_, t, kti, c0, cw, cb, name) = job
            cp(cast_engs[st_ % 2], stb[i][:, 0:cw], stg[i][:, 0:cw])
            t0 = c0 // cb
            ntl = cw // cb
            dma(t[t0:t0 + ntl, :, kti, :].rearrange("t p c -> p t c"),
                stb[i][:, 0:cw].rearrange("p (t c) -> p t c", c=cb), sem=f"stb{i}",
                w=[("scr", name, tt_, kti) for tt_ in range(t0, t0 + ntl)])

        for (name, src, lyr) in conv_jobs:
            if lyr >= DEPTH or (DEFER and lyr >= 1):
                continue
            t, kt, nt, cb = scr[name]
            N = nt * cb
            CW = (2048 // cb) * cb
            for kti in range(kt):
                for c0 in range(0, N, CW):
                    cw = min(CW, N - c0)
                    i = step % NST
                    if len(pend) >= NST - 1:
                        finish(pend.pop(0))
                    dma(stg[i][:, 0:cw], src[kti * 128:(kti + 1) * 128, c0:c0 + cw], sem=f"stg{i}")
                    pend.append((i, step, t, kti, c0, cw, cb, name))
                    step += 1
        while pend:
            finish(pend.pop(0))
        A.release(mk)

    NSTB = 4
    dstg = [A.alloc([1024], F32) for _ in range(NSTB)] if DEFER else []
    dstb = [A.alloc([1024], BF16) for _ in range(NSTB)] if DEFER else []
    dsteps = []
    if DEFER and not skip_conv and DEPTH > 1:
        for (name, src, lyr) in conv_jobs:
            if lyr < 1:
                continue
            t, kt, nt, cb = scr[name]
            N = nt * cb
            CW = (1024 // cb) * cb
            for kti in range(kt):
                for c0 in range(0, N, CW):
                    dsteps.append((name, src, t, kti, c0, min(CW, N - c0), cb))
    dstate = dict(next=0, pend=[])

    def dfinish(job):
        (i, k, name, src, t, kti, c0, cw, cb) = job
        cp("dve" if k % 2 else "act", dstb[i][:, 0:cw], dstg[i][:, 0:cw])
        t0 = c0 // cb
        ntl = cw // cb
        dma(t[t0:t0 + ntl, :, kti, :].rearrange("t p c -> p t c"),
            dstb[i][:, 0:cw].rearrange("p (t c) -> p t c", c=cb), sem=f"dstb{i}",
            w=[("scr", name, tt_, kti) for tt_ in range(t0, t0 + ntl)])

    def emit_conv(nsteps, flush=False):
        for _ in range(nsteps):
            k = dstate["next"]
            if k >= len(dsteps):
                break
            dstate["next"] = k + 1
            (name, src, t, kti, c0, cw, cb) = dsteps[k]
            i = k % NSTB
            if len(dstate["pend"]) >= NSTB - 1:
                dfinish(dstate["pend"].pop(0))
            dma(dstg[i][:, 0:cw], src[kti * 128:(kti + 1) * 128, c0:c0 + cw], sem=f"dstg{i}")
            dstate["pend"].append((i, k, name, src, t, kti, c0, cw, cb))
        if flush:
            while dstate["pend"]:
                dfinish(dstate["pend"].pop(0))

    rings = {}

    def wring(cls, shape, nbuf):
        rings[cls] = dict(bufs=[A.alloc(shape, BF16) for _ in range(nbuf)], i=0)

    def wload(cls, name, ti, slot=None):
        rg = rings[cls]
        if slot is None:
            i = rg["i"]
            rg["i"] = (i + 1) % len(rg["bufs"])
        else:
            i = slot
        buf = rg["bufs"][i]
        t, kt, nt, cb = scr[name]
        dst = buf[:, 0:kt, 0:cb]
        dma(dst, t[ti], sem=f"w_{cls}_{i}", r=[("scr", name, ti, k) for k in range(kt)])
        return dst

    c = {}
    c["Ere"] = A.alloc([12, LS5], BF16)
    c["Eim"] = A.alloc([12, LS5], BF16)
    c["Bre"] = A.alloc([12, 128], BF16)
    c["Bim"] = A.alloc([12, 128], BF16)
    c["Cre"] = A.alloc([12, 128], BF16)
    c["Cimn"] = A.alloc([12, 128], BF16)
    c["Dd"] = A.alloc([3, 128], BF16, align=256)
    c["r"] = A.alloc([12], F32, align=64)
    c["rtab"] = A.alloc([12, LS5], F32)
    c["Wa"] = A.alloc([3, 128], F32)
    c["Wx"] = A.alloc([3, 128], F32)
    c["clru"] = A.alloc([3], F32, align=64)
    c["kT"] = A.alloc([3, T], BF16)
    c["vc"] = A.alloc([16, W], BF16)
    c["slre"] = A.alloc([12], F32, align=64)
    c["slim"] = A.alloc([12], F32, align=64)
    c["hst"] = A.alloc([3], F32, align=64)
    c["halo"] = A.alloc([3, 4], F32, align=64)

    negones = A.alloc([128], F32)
    memset("pool", negones, -1.0)

    xTs = [A.alloc([8, NCHUNK], F32)]
    hn = A.alloc([8, NCHUNK], BF16)
    GELU = AF.Gelu_apprx_tanh

    def prep_layer(l):
        mk = A.mark()
        sm = lambda: A.alloc([12], F32, align=64)
        lr = pcol("lam_re", l, 0, 12)
        li = pcol("lam_im", l, 0, 12)
        ldt = pcol("log_dt", l, 0, 12)
        dt = sm(); lrd = sm(); th = sm(); ea = c["r"]; tq = sm(); th2 = sm()
        sn = sm(); cs = sm(); are = sm(); aim = sm(); den = sm(); rden = sm(); nr = sm()
        cre = sm(); cim = sm(); t1 = sm(); t2 = sm()
        act(dt, ldt, AF.Exp)
        tt("dve", lrd, lr, dt, ALU.mult)
        tt("dve", th, li, dt, ALU.mult)
        act(ea, lrd, AF.Exp)
        for Pp in (64 * PI, 32 * PI, 16 * PI, 8 * PI, 4 * PI, 2 * PI):
            ts("dve", tq, th, Pp - PI, ALU.is_ge, Pp, ALU.mult)
            tt("dve", th, th, tq, ALU.subtract)
        ts("dve", th2, th, PI / 2, ALU.add)
        ts("dve", tq, th2, PI, ALU.is_ge, 2 * PI, ALU.mult)
        tt("dve", th2, th2, tq, ALU.subtract)
        act(sn, th, AF.Sin)
        act(cs, th2, AF.Sin)
        tt("dve", are, ea, cs, ALU.mult)
        tt("dve", aim, ea, sn, ALU.mult)
        tt("dve", t1, lr, lr, ALU.mult)
        tt("dve", t2, li, li, ALU.mult)
        tt("dve", den, t1, t2, ALU.add)
        P.op("dve", lambda e, rden=rden, den=den: e.reciprocal(out=rden, in_=den), r=[den], w=[rden])
        ts("dve", nr, are, -1.0, ALU.add)
        tt("dve", t1, nr, lr, ALU.mult)
        tt("dve", t2, aim, li, ALU.mult)
        tt("dve", t1, t1, t2, ALU.add)
        tt("dve", cre, t1, rden, ALU.mult)
        tt("dve", t1, aim, lr, ALU.mult)
        tt("dve", t2, nr, li, ALU.mult)
        tt("dve", t1, t1, t2, ALU.subtract)
        tt("dve", cim, t1, rden, ALU.mult)
        Ere = A.alloc([12, LS5], F32)
        Eim = A.alloc([12, LS5], F32)
        cp("dve", Ere[:, :, 0:1], cs.unsqueeze(2))
        cp("dve", Eim[:, :, 0:1], sn.unsqueeze(2))
        ta = A.alloc([12, 64], F32)
        tb = A.alloc([12, 64], F32)
        k = 1
        while k < LS5:
            ck = Ere[:, :, k - 1:k].to_broadcast([128, 12, k])
            sk = Eim[:, :, k - 1:k].to_broadcast([128, 12, k])
            tt("dve", ta[:, :, 0:k], Ere[:, :, 0:k], ck, ALU.mult)
            tt("dve", tb[:, :, 0:k], Eim[:, :, 0:k], sk, ALU.mult)
            tt("dve", Ere[:, :, k:2 * k], ta[:, :, 0:k], tb[:, :, 0:k], ALU.subtract)
            tt("dve", ta[:, :, 0:k], Ere[:, :, 0:k], sk, ALU.mult)
            tt("dve", tb[:, :, 0:k], Eim[:, :, 0:k], ck, ALU.mult)
            tt("dve", Eim[:, :, k:2 * k], ta[:, :, 0:k], tb[:, :, 0:k], ALU.add)
            k *= 2
        memset("pool", c["rtab"], 0.0)
        cp("pool", c["rtab"][:, :, 1:LS5], c["r"].unsqueeze(2).to_broadcast([128, 12, LS5 - 1]))
        cp("dve", c["Ere"], Ere)
        cp("dve", c["Eim"], Eim)
        if l == 0:
            dump("Ere", Ere.rearrange("p a b -> p (a b)"), [128, 12 * LS5])
            dump("Eim", Eim.rearrange("p a b -> p (a b)"), [128, 12 * LS5])
            dump("r", c["r"], [128, 12])
            dump("cre", cre, [128, 12])
            dump("cim", cim, [128, 12])
        bre = A.alloc([12, 16], F32)
        bim = A.alloc([12, 16], F32)
        dma(bre, s5_b_re[l].rearrange("(m two) q h -> (two q) m h", two=2), sem="prep")
        dma(bim, s5_b_im[l].rearrange("(m two) q h -> (two q) m h", two=2), sem="prep")
        Bbr = A.alloc([12, 16], F32)
        Bbi = A.alloc([12, 16], F32)
        u1 = A.alloc([12, 16], F32)
        u2 = A.alloc([12, 16], F32)
        creb = cre.unsqueeze(2).to_broadcast([128, 12, 16])
        cimb = cim.unsqueeze(2).to_broadcast([128, 12, 16])
        tt("dve", u1, bre, creb, ALU.mult)
        tt("dve", u2, bim, cimb, ALU.mult)
        tt("dve", Bbr, u1, u2, ALU.subtract)
        tt("dve", u1, bim, creb, ALU.mult)
        tt("dve", u2, bre, cimb, ALU.mult)
        tt("dve", Bbi, u1, u2, ALU.add)
        X = A.alloc([12, 128], F32)
        for (src_, dstname) in ((Bbr, "Bre"), (Bbi, "Bim")):
            memset("pool", X, 0.0)
            for m in range(12):
                for two in range(2):
                    g = 2 * m + two
                    c0 = 16 * (g % 8)
                    cp("pool", X[two * 64:(two + 1) * 64, m, c0:c0 + 16], src_[two * 64:(two + 1) * 64, m, :])
            for m4 in range(3):
                pb = nb()
                for mi in range(4):
                    tr(pb[:, mi * 128:(mi + 1) * 128], X[:, m4 * 4 + mi, :], ident_f)
                cp("dve", c[dstname][:, m4 * 4:(m4 + 1) * 4, :], pb.rearrange("p (a b) -> p a b", a=4))
        for (srcC, dstname, scale) in ((s5_c_re, "Cre", 1.0), (s5_c_im, "Cimn", -1.0)):
            memset("pool", c[dstname], 0.0)
            for yt in range(3):
                ct = X[:, yt, :]
                srcv = srcC[l].rearrange("(y g) h q -> y (g h) q", g=8)[yt]
                dma(ct[:, 0:64], srcv, sem="prep")
                dma(ct[:, 64:128], srcv, sem="prep")
                pb = nb()
                tr(pb[:, 0:128], ct, ident_f)
                for mi in range(4):
                    m = 4 * yt + mi
                    for two in range(2):
                        g = 2 * m + two
                        c0 = 16 * (g % 8)
                        ts("dve", c[dstname][two * 64:(two + 1) * 64, m, c0:c0 + 16],
                           pb[two * 64:(two + 1) * 64, c0:c0 + 16], scale, ALU.mult)
        for yt in range(3):
            ts("dve", c["Dd"][:, yt, :], ident_f, pcol("s5_d", l, yt), ALU.mult)
        memset("pool", c["Wa"], 0.0)
        memset("pool", c["Wx"], 0.0)
        for kt in range(3):
            for half in range(2):
                hsl = slice(half * 64, (half + 1) * 64)
                dma(c["Wa"][hsl, kt, half * 64:(half + 1) * 64], lru_w_a[l, 2 * kt + half], sem="prep")
                dma(c["Wx"][hsl, kt, half * 64:(half + 1) * 64], lru_w_x[l, 2 * kt + half], sem="prep")
        t3 = A.alloc([3], F32, align=64)
        act(t3, pcol("lam", l, 0, 3), AF.Exp, scale=-1.0)
        act(t3, t3, AF.Ln, bias=1.0)
        ts("dve", c["clru"], t3, -8.0, ALU.mult)
        A.release(mk)

    def rmsnorm(xT, gname, l, out_bf=None, out_f=None):
        mk = A.mark()
        sq = [A.alloc([NCHUNK], BF16), A.alloc([NCHUNK], BF16)]
        std = A.alloc([NCHUNK], F32)
        rstd = A.alloc([NCHUNK], F32)
        pb = nb()
        for dk in range(8):
            act(sq[dk % 2], xT[:, dk, :], AF.Square)
            mm(pb, ones_b, sq[dk % 2], dk == 0, dk == 7)
        act(std, pb, AF.Sqrt, scale=1.0 / D, bias=1e-6)
        P.op("dve", lambda e: e.reciprocal(out=rstd, in_=std), r=[std], w=[rstd])
        for dk in range(8):
            if out_bf is not None:
                stt(out_bf[:, dk, :], xT[:, dk, :], pcol(gname, l, dk), rstd, ALU.mult, ALU.mult)
            if out_f is not None:
                stt(out_f[:, dk, :], xT[:, dk, :], pcol(gname, l, dk), rstd, ALU.mult, ALU.mult)
        A.release(mk)

    def mixer(l, b, c_, xT, first):
        t0 = c_ * NCHUNK
        mkL = A.mark()
        wring("win", [8, 384], 3)
        wring("wbr", [3, 256], 3)
        wring("wout", [8, 256], 2)
        wring("glu", [3, 384], 1)
        rmsnorm(xT, "mix_g", l, out_bf=hn)
        if first:
            dump("hn", hn.rearrange("p a b -> p (a b)"), [128, 8 * NCHUNK])
        uT = A.alloc([3, NCHUNK], BF16)
        qT = A.alloc([3, NCHUNK], BF16)
        xl = A.alloc([3, NCHUNK + 4], F32)
        gy = A.alloc([3, NCHUNK], BF16)
        a1 = A.alloc([3, NCHUNK], BF16)
        aT = A.alloc([3, NCHUNK], BF16)
        bT = A.alloc([3, NCHUNK], BF16)
        cT = A.alloc([3, NCHUNK], BF16)
        kT, vc = c["kT"], c["vc"]
        if c_ == 0:
            memset("pool", xl[:, :, 0:3], 0.0)
        else:
            cp("pool", xl[:, :, 0:3], c["halo"][:, :, 0:3])
        for wt in range(6):
            wtile = wload("win", f"win{l}", wt)
            if wt == 3:
                for tt_ in range(4):
                    pb = nb()
                    for dk in range(8):
                        mm(pb[:, 0:W], hn[:, dk, tt_ * 128:(tt_ + 1) * 128], wtile[:, dk, :], dk == 0, dk == 7)
                    cp("act" if tt_ % 2 else "dve", vc[:, 4 * c_ + tt_, :], pb[:, 0:W])
                continue
            for j in range(3):
                pb = nb()
                for dk in range(8):
                    mm(pb, wtile[:, dk, j * 128:(j + 1) * 128], hn[:, dk, :], dk == 0, dk == 7)
                if wt == 0:
                    cp("act", uT[:, j, :], pb)
                elif wt == 1:
                    act(qT[:, j, :], pb, AF.Copy, scale=0.125)
                elif wt == 2:
                    cp("dve", kT[:, j, t0:t0 + NCHUNK], pb)
                elif wt == 4:
                    cp("dve", xl[:, j, 3:3 + NCHUNK], pb)
                elif wt == 5:
                    act(gy[:, j, :], pb, GELU)
        cp("pool", c["halo"][:, :, 0:3], xl[:, :, NCHUNK:NCHUNK + 3])
        if l == 0:
            emit_conv(CONV_SLICE)
        if first:
            dump("uT", uT.rearrange("p a b -> p (a b)"), [128, 3 * NCHUNK])
            dump("qT", qT.rearrange("p a b -> p (a b)"), [128, 3 * NCHUNK])
            dump("vc", vc[:, 0:4, :].rearrange("p a b -> p (a b)"), [128, 4 * W])
        mk = A.mark()
        NSET = 3
        S5T = [[A.alloc([4, LS5], F32) for _ in range(4)] for _ in range(NSET)]
        S5B = [[A.alloc([4, LS5], BF16) for _ in range(2)] for _ in range(NSET)]
        rin = [[A.alloc([4], F32, align=64) for _ in range(2)] for _ in range(NSET)]
        fl = lambda v: v.rearrange("p a b -> p (a b)")
        xc = A.alloc([NCHUNK], F32); av = A.alloc([NCHUNK], F32); ig = A.alloc([NCHUNK], F32)
        om = A.alloc([NCHUNK], F32); hh = A.alloc([NCHUNK], F32)

        s5_R = [psb[3], psb[5]]
        s5_I = [psb[4], psb[6]]
        s5_Y = psb[7]

        def s5_A(it, sc, yt):
            o = sc * LS5
            b1, b2, b3, b4 = S5T[it % NSET]
            rn = rin[it % NSET]
            m0 = 4 * yt
            R = s5_R[it % 2].rearrange("p (a b) -> p a b", a=4)
            I = s5_I[it % 2].rearrange("p (a b) -> p a b", a=4)
            for mi in range(4):
                mm(R[:, mi, :], c["Bre"][:, m0 + mi, :], uT[:, yt, o:o + LS5], True, True)
                mm(I[:, mi, :], c["Bim"][:, m0 + mi, :], uT[:, yt, o:o + LS5], True, True)
            Er4 = c["Ere"][:, m0:m0 + 4, :]
            Ei4 = c["Eim"][:, m0:m0 + 4, :]
            tt("dve", b1, R, Er4, ALU.mult)
            tt("dve", b2, I, Ei4, ALU.mult)
            tt("dve", b3, I, Er4, ALU.mult)
            tt("dve", b4, R, Ei4, ALU.mult)
            tt("dve", b1, b1, b2, ALU.add)
            tt("pool", b3, b3, b4, ALU.subtract)
            if not (c_ == 0 and sc == 0):
                tt("dve", rn[0], c["r"][:, m0:m0 + 4], c["slre"][:, m0:m0 + 4], ALU.mult)
                tt("dve", rn[1], c["r"][:, m0:m0 + 4], c["slim"][:, m0:m0 + 4], ALU.mult)
                tt("dve", b1[:, :, 0:1], b1[:, :, 0:1], rn[0].unsqueeze(2), ALU.add)
                tt("dve", b3[:, :, 0:1], b3[:, :, 0:1], rn[1].unsqueeze(2), ALU.add)

        def s5_B(it, sc, yt):
            b1, b2, b3, b4 = S5T[it % NSET]
            sbf = S5B[it % NSET]
            m0 = 4 * yt
            Er4 = c["Ere"][:, m0:m0 + 4, :]
            Ei4 = c["Eim"][:, m0:m0 + 4, :]
            rt4 = c["rtab"][:, m0:m0 + 4, :]
            for (z_, w_) in ((b2, b1), (b4, b3)):
                P.op("dve", lambda e, z_=z_, w_=w_, rt4=rt4: e.tensor_tensor_scan(
                    out=fl(z_), data0=fl(rt4), data1=fl(w_), initial=0.0, op0=ALU.mult, op1=ALU.add),
                    r=[w_, rt4], w=[z_])
            tt("dve", b1, b2, Er4, ALU.mult)
            tt("pool", b3, b4, Ei4, ALU.mult)
            tt("pool", sbf[0], b1, b3, ALU.subtract)
            tt("dve", c["slre"][:, m0:m0 + 4].unsqueeze(2), b1[:, :, LS5 - 1:LS5], b3[:, :, LS5 - 1:LS5], ALU.subtract)
            tt("pool", b1, b4, Er4, ALU.mult)
            tt("pool", b3, b2, Ei4, ALU.mult)
            tt("pool", sbf[1], b1, b3, ALU.add)
            tt("dve", c["slim"][:, m0:m0 + 4].unsqueeze(2), b1[:, :, LS5 - 1:LS5], b3[:, :, LS5 - 1:LS5], ALU.add)

        def s5_C(it, sc, yt):
            o = sc * LS5
            sbf = S5B[it % NSET]
            m0 = 4 * yt
            q = it % 4
            Y = s5_Y[:, q * LS5:(q + 1) * LS5]
            mm(Y, c["Dd"][:, yt, :], uT[:, yt, o:o + LS5], True, False)
            for mi in range(4):
                mm(Y, c["Cre"][:, m0 + mi, :], sbf[0][:, mi, :], False, False)
                mm(Y, c["Cimn"][:, m0 + mi, :], sbf[1][:, mi, :], False, mi == 3)
            act(a1[:, yt, o:o + LS5], Y, GELU)

        def lru_step(kt):
            ts("dve", xc, xl[:, kt, 0:NCHUNK], pcol("conv_w", l, 0 * 3 + kt), ALU.mult, pcol("conv_b", l, kt), ALU.add)
            for k in range(1, 4):
                stt(xc, xl[:, kt, k:k + NCHUNK], pcol("conv_w", l, k * 3 + kt), xc, ALU.mult, ALU.add)
            pa = psb[1]
            px = psb[2]
            mm(pa, c["Wa"][:, kt, :], xc, True, True)
            mm(px, c["Wx"][:, kt, :], xc, True, True)
            act(av, pa, AF.Sigmoid, bias=pcol("b_a", l, kt))
            act(av, av, AF.Exp, scale=c["clru"][:, kt:kt + 1])
            act(ig, px, AF.Sigmoid, bias=pcol("b_x", l, kt))
            tt("pool", om, av, av, ALU.mult)
            ts("pool", om, om, -1.0, ALU.mult, 1.0, ALU.add)
            act(om, om, AF.Sqrt)
            tt("pool", ig, ig, xc, ALU.mult)
            tt("pool", om, om, ig, ALU.mult)
            if c_ == 0:
                P.op("dve", lambda e: e.tensor_tensor_scan(
                    out=hh, data0=av, data1=om, initial=0.0, op0=ALU.mult, op1=ALU.add), r=[av, om], w=[hh])
            else:
                ini = c["hst"][:, kt:kt + 1]
                P.op("dve", lambda e, ini=ini: e.tensor_tensor_scan(
                    out=hh, data0=av, data1=om, initial=ini, op0=ALU.mult, op1=ALU.add), r=[av, om, ini], w=[hh])
            cp("dve", c["hst"][:, kt:kt + 1], hh[:, NCHUNK - 1:NCHUNK])
            tt("pool", cT[:, kt, :], hh, gy[:, kt, :], ALU.mult)

        steps = [(sc * 3 + yt, sc, yt) for sc in range(NCHUNK // LS5) for yt in range(3)]
        ns = len(steps)
        for k in range(ns + 2):
            if k < ns:
                s5_A(*steps[k])
            if 0 <= k - 1 < ns:
                s5_B(*steps[k - 1])
            if 0 <= k - 2 < ns:
                s5_C(*steps[k - 2])
            if k in (3, 7, 11):
                lru_step(k // 4)
        if l == 0 and DEFER:
            emit_conv(CONV_SLICE)
        wg = wload("glu", f"glu{l}", 0)
        sg = [A.alloc([NCHUNK], BF16), A.alloc([NCHUNK], BF16)]
        for j in range(3):
            pb = nb()
            for kt in range(3):
                mm(pb, wg[:, kt, j * 128:(j + 1) * 128], a1[:, kt, :], kt == 0, kt == 2)
            act(sg[j % 2], pb, AF.Sigmoid, bias=pcol("b_glu", l, j))
            tt("pool", aT[:, j, :], a1[:, j, :], sg[j % 2], ALU.mult)
        A.release(mk)
        if first:
            dump("a1", a1.rearrange("p a b -> p (a b)"), [128, 3 * NCHUNK])
            dump("aT", aT.rearrange("p a b -> p (a b)"), [128, 3 * NCHUNK])
            dump("cT", cT.rearrange("p a b -> p (a b)"), [128, 3 * NCHUNK])
        mk = A.mark()
        NPIPE = 2
        ex_ = [[A.alloc([NCHUNK], BF16) for _ in range(2)] for _ in range(NPIPE)]
        sp_ = ex_
        ar_ = [[A.alloc([NCHUNK], F32) for _ in range(2)] for _ in range(NPIPE)]
        ww_ = [[A.alloc([NCHUNK], BF16) for _ in range(2)] for _ in range(NPIPE)]
        carry = [A.alloc([NCHUNK], F32) for _ in range(2)]
        zb = [[psb[0], psb[1]], [psb[2], psb[3]]]
        ob = psb[4]
        csb = [psb[5], psb[6]]
        items = []
        for hp in range(3):
            for B in range(4 * c_ + 3, -1, -1):
                items.append((hp, B))

        def s1(n):
            hp, B = items[n]
            for hh_ in range(2):
                ps_ = slice(hh_ * 64, (hh_ + 1) * 64)
                mm(zb[n % 2][hh_], kT[ps_, hp, B * 128:(B + 1) * 128], qT[ps_, hp, :], True, False)

        s1(0)
        for n, (hp, B) in enumerate(items):
            if n + 1 < len(items):
                s1(n + 1)
            pi = n % NPIPE
            diag = B >= 4 * c_
            lastB = (B == 0)
            firstB = (B == 4 * c_ + 3)
            for hh_ in range(2):
                h = 2 * hp + hh_
                z = zb[n % 2][hh_]
                e_ = ex_[pi][hh_]; s_ = sp_[pi][hh_]; a_ = ar_[pi][hh_]; w_ = ww_[pi][hh_]
                act(e_, z, AF.Exp)
                act(s_, e_, AF.Ln, bias=1.0)
                if diag:
                    tt("pool", s_, s_, masks[:, B - 4 * c_, :], ALU.mult)
                mm(z, trineg, s_, False, True)
                if firstB:
                    cp("dve", a_, z)
                else:
                    tt("dve", a_, z, carry[hh_], ALU.subtract)
                if not lastB:
                    mm(csb[hh_], ones_b, s_, True, True)
                    if firstB:
                        cp("dve", carry[hh_], csb[hh_])
                    else:
                        tt("dve", carry[hh_], csb[hh_], carry[hh_], ALU.add)
                act(w_, a_, AF.Exp)
                if diag:
                    tt("pool", w_, w_, masks[:, B - 4 * c_, :], ALU.mult)
                mm(ob[hh_ * 64:(hh_ + 1) * 64, :], vc[:, B, h * 64:(h + 1) * 64], w_, firstB, lastB)
            if lastB:
                cp("act", bT[:, hp, :], ob)
        A.release(mk)
        if first:
            dump("bT", bT.rearrange("p a b -> p (a b)"), [128, 3 * NCHUNK])
        if l == 0:
            emit_conv(CONV_SLICE)
        mk = A.mark()
        merged = A.alloc([8, NCHUNK], BF16)
        gte = [A.alloc([NCHUNK], F32) for _ in range(2)]
        macc = A.alloc([NCHUNK], F32)
        mtmp = A.alloc([NCHUNK], F32)
        branches = [aT, bT, cT]
        gi = 0
        wbr_t = {}
        win_g = {}
        for dt_ in range(8):
            for n in range(3):
                if dt_ % 2 == 0:
                    wbr_t[n] = wload("wbr", f"wbr{l}_{n}", dt_ // 2, slot=n)
                col = n * D + dt_ * 128
                gtile = 6 + col // 384
                goff = col % 384
                if win_g.get(n, (None, None))[0] != gtile:
                    win_g[n] = (gtile, wload("win", f"win{l}", gtile, slot=n))
                wgt = win_g[n][1]
                pbr = nb()
                for kt in range(3):
                    mm(pbr, wbr_t[n][:, kt, (dt_ % 2) * 128:(dt_ % 2 + 1) * 128], branches[n][:, kt, :], kt == 0, kt == 2)
                pg = nb()
                for dk in range(8):
                    mm(pg, wgt[:, dk, goff:goff + 128], hn[:, dk, :], dk == 0, dk == 7)
                g_ = gte[gi % 2]
                gi += 1
                act(g_, pg, AF.Sigmoid, bias=pcol("gate_b", l, n * 8 + dt_))
                if n == 0:
                    tt("dve", macc, pbr, g_, ALU.mult)
                elif n == 1:
                    tt("dve", mtmp, pbr, g_, ALU.mult)
                    tt("pool", macc, macc, mtmp, ALU.add)
                else:
                    tt("dve", mtmp, pbr, g_, ALU.mult)
                    tt("pool", merged[:, dt_, :], macc, mtmp, ALU.add)
        if first:
            dump("merged", merged.rearrange("p a b -> p (a b)"), [128, 8 * NCHUNK])
        for d2 in range(8):
            if d2 % 2 == 0:
                wo = wload("wout", f"wout{l}", d2 // 2)
            pb = nb()
            for dk in range(8):
                mm(pb, wo[:, dk, (d2 % 2) * 128:(d2 % 2 + 1) * 128], merged[:, dk, :], dk == 0, dk == 7)
            tt("dve", xT[:, d2, :], pb, xT[:, d2, :], ALU.add)
        A.release(mk)
        A.release(mkL)
        if first:
            dump("x1", xT.rearrange("p a b -> p (a b)"), [128, 8 * NCHUNK])

    def ffn_dense(l, xT):
        mkF = A.mark()
        wring("gu", [8, 256], 4)
        wring("wd", [28, 128], 3)
        rmsnorm(xT, "ffn_g", l, out_bf=hn)
        nft = DFF // 128
        actb = A.alloc([nft, NCHUNK], BF16)
        sgb = [A.alloc([NCHUNK], BF16) for _ in range(2)]
        for ft in range(nft):
            if ft % 2 == 0:
                wg_ = wload("gu", "fg", ft // 2)
                wu_ = wload("gu", "fu", ft // 2)
            pg = nb(); pu = nb()
            cs_ = slice((ft % 2) * 128, (ft % 2 + 1) * 128)
            for dk in range(8):
                mm(pg, wg_[:, dk, cs_], hn[:, dk, :], dk == 0, dk == 7)
            for dk in range(8):
                mm(pu, wu_[:, dk, cs_], hn[:, dk, :], dk == 0, dk == 7)
            act(sgb[ft % 2], pg, AF.Silu)
            tt("dve", actb[:, ft, :], pu, sgb[ft % 2], ALU.mult)
        for d2 in range(8):
            wd_ = wload("wd", "fd", d2)
            pb = nb()
            for ft in range(nft):
                mm(pb, wd_[:, ft, :], actb[:, ft, :], ft == 0, ft == nft - 1)
            tt("dve", xT[:, d2, :], pb, xT[:, d2, :], ALU.add)
        A.release(mkF)

    def ffn_moe(l, xT, first):
        mkF = A.mark()
        wring("gu", [8, 256], 4)
        wring("wd", [28, 128], 3)
        combbc = A.alloc([8, NCHUNK], BF16)
        comb = A.alloc([4, 8], F32, align=64)
        mkR = A.mark()
        hnf = A.alloc([8, NCHUNK], F32)
        rmsnorm(xT, "ffn_g", l, out_bf=hn, out_f=hnf)
        lg = A.alloc([4, 8], F32, align=64)
        lg2 = A.alloc([4, 8], F32, align=64)
        eq1 = A.alloc([4, 8], F32, align=64)
        eq2 = A.alloc([4, 8], F32, align=64)
        m1 = A.alloc([4], F32, align=64); m2 = A.alloc([4], F32, align=64)
        dd = A.alloc([4], F32, align=64); w1 = A.alloc([4], F32, align=64); w2 = A.alloc([4], F32, align=64)
        lcol = [A.alloc([128], F32) for _ in range(2)]
        pl = nb()
        for tt_ in range(4):
            for dk in range(8):
                mm(pl[:, tt_ * 8:(tt_ + 1) * 8], hnf[:, dk, tt_ * 128:(tt_ + 1) * 128], rw[:, dk, :], dk == 0, dk == 7)
        tt("dve", lg, pl[:, 0:32].rearrange("p (a b) -> p a b", a=4), rb_bc.unsqueeze(1).to_broadcast([128, 4, 8]), ALU.add)
        P.op("dve", lambda e, m1=m1, lg=lg: e.tensor_reduce(out=m1, in_=lg, axis=mybir.AxisListType.X, op=ALU.max), r=[lg], w=[m1])
        tt("dve", eq1, lg, m1.unsqueeze(2).to_broadcast([128, 4, 8]), ALU.is_equal)
        stt(lg2, eq1, -1e30, lg, ALU.mult, ALU.add)
        P.op("dve", lambda e, m2=m2, lg2=lg2: e.tensor_reduce(out=m2, in_=lg2, axis=mybir.AxisListType.X, op=ALU.max), r=[lg2], w=[m2])
        tt("dve", eq2, lg2, m2.unsqueeze(2).to_broadcast([128, 4, 8]), ALU.is_equal)
        tt("dve", dd, m2, m1, ALU.subtract)
        act(dd, dd, AF.Exp)
        ts("dve", w1, dd, 1.0, ALU.add)
        P.op("dve", lambda e, w1=w1: e.reciprocal(out=w1, in_=w1), r=[w1], w=[w1])
        tt("dve", w2, dd, w1, ALU.mult)
        tt("dve", eq1, eq1, w1.unsqueeze(2).to_broadcast([128, 4, 8]), ALU.mult)
        tt("dve", eq2, eq2, w2.unsqueeze(2).to_broadcast([128, 4, 8]), ALU.mult)
        tt("dve", comb, eq1, eq2, ALU.add)
        if first:
            dump("comb", comb.rearrange("p a b -> p (a b)"), [128, 32])
        ci = 0
        for e_ in range(NE):
            pbc = nb()
            for tt_ in range(4):
                lc_ = lcol[ci % 2]
                ci += 1
                ts("dve", lc_, ones_f, comb[:, tt_, e_:e_ + 1], ALU.mult)
                mm(pbc[:, tt_ * 128:(tt_ + 1) * 128], lc_, ident_f, True, True)
            cp("act", combbc[:, e_, :], pbc)
        A.release(mkR)
        nft = DFE // 128
        actb = A.alloc([nft, NCHUNK], BF16)
        sgb = [A.alloc([NCHUNK], BF16) for _ in range(2)]
        sgc = [A.alloc([NCHUNK], BF16) for _ in range(2)]
        for e_ in range(NE):
            for ft in range(nft):
                if ft % 2 == 0:
                    wg_ = wload("gu", f"mg{e_}", ft // 2)
                    wu_ = wload("gu", f"mu{e_}", ft // 2)
                pg = nb(); pu = nb()
                cs_ = slice((ft % 2) * 128, (ft % 2 + 1) * 128)
                for dk in range(8):
                    mm(pg, wg_[:, dk, cs_], hn[:, dk, :], dk == 0, dk == 7)
                for dk in range(8):
                    mm(pu, wu_[:, dk, cs_], hn[:, dk, :], dk == 0, dk == 7)
                act(sgb[ft % 2], pg, AF.Silu)
                tt("pool", sgc[ft % 2], sgb[ft % 2], combbc[:, e_, :], ALU.mult)
                tt("dve", actb[:, ft, :], pu, sgc[ft % 2], ALU.mult)
            for d2 in range(8):
                wd_ = wload("wd", f"md{e_}", d2)
                pb = nb()
                for ft in range(nft):
                    mm(pb, wd_[:, ft, :], actb[:, ft, :], ft == 0, ft == nft - 1)
                tt("dve", xT[:, d2, :], pb, xT[:, d2, :], ALU.add)
        A.release(mkF)

    it_ = 0
    CONV_SLICE = (len(dsteps) + NSEQ * NCH * 4 - 1) // (NSEQ * NCH * 4)
    for l in range(DEPTH):
        if l == 1:
            emit_conv(len(dsteps), flush=True)
        prep_layer(l)
        for b in range(NSEQ):
            for c_ in range(NCH):
                t0 = c_ * NCHUNK
                first = (b == 0 and c_ == 0 and l == 0)
                xT = xTs[0]
                it_ += 1
                if l == 0:
                    mk0 = A.mark()
                    xs = [A.alloc([D], F32) for _ in range(4)]
                    for tt_ in range(4):
                        dma(xs[tt_], x[b, t0 + tt_ * 128:t0 + (tt_ + 1) * 128, :], sem=f"xs{tt_}")
                    for dk in range(8):
                        pb = nb()
                        for tt_ in range(4):
                            tr(pb[:, tt_ * 128:(tt_ + 1) * 128], xs[tt_][:, dk * 128:(dk + 1) * 128], ident_f)
                        cp("act" if dk % 2 else "dve", xT[:, dk, :], pb)
                    A.release(mk0)
                else:
                    dma(xT, xmid[b, c_], sem=f"xm{it_ % 2}", r=[("xmid", b, c_)])
                mixer(l, b, c_, xT, first)
                if l == 0:
                    emit_conv(CONV_SLICE)
                if l % 2 == 0:
                    ffn_dense(l, xT)
                else:
                    ffn_moe(l, xT, b == 0 and c_ == 0)
                if first:
                    dump("x2", xT.rearrange("p a b -> p (a b)"), [128, 8 * NCHUNK])
                if l < DEPTH - 1:
                    dma(xmid[b, c_], xT, sem=f"xw{it_ % 2}", w=[("xmid", b, c_)])
                else:
                    mk = A.mark()
                    yT = A.alloc([8, NCHUNK], F32)
                    rmsnorm(xT, "fin_g", 0, out_f=yT)
                    ot = [A.alloc([D], F32) for _ in range(4)]
                    for tt_ in range(4):
                        for half in range(2):
                            pb = nb()
                            for q in range(4):
                                dk = half * 4 + q
                                tr(pb[:, q * 128:(q + 1) * 128], yT[:, dk, tt_ * 128:(tt_ + 1) * 128], ident_f)
                            cp("act" if half else "dve", ot[tt_][:, half * 512:(half + 1) * 512], pb)
                        dma(out[b, t0 + tt_ * 128:t0 + (tt_ + 1) * 128, :], ot[tt_], sem=f"ot{tt_}",
                            w=[("out", b, c_, tt_)], final=True)
                    A.release(mk)

    P.emit(st)
    st.close()
    return nc, dbg_out, A.peak


_WNAMES = ["mix_norm_g", "w_in", "gate_b", "s5_lambda_re", "s5_lambda_im", "s5_log_dt", "s5_b_re", "s5_b_im",
           "s5_c_re", "s5_c_im", "s5_d", "s5_w_glu", "s5_b_glu", "conv_w", "conv_b", "lru_w_a", "lru_b_a",
           "lru_w_x", "lru_b_x", "lru_lambda", "w_branch", "w_out", "ffn_norm_g", "ffn_w_gate", "ffn_w_up",
           "ffn_w_down", "router_w", "router_b", "moe_w_gate", "moe_w_up", "moe_w_down", "final_norm_g"]


def kernel(**inputs):
    x = np.ascontiguousarray(np.asarray(inputs["x"], dtype=np.float32))
    ws = {k: np.ascontiguousarray(np.asarray(inputs[k], dtype=np.float32)) for k in _WNAMES}
    nc, _, _ = build()
    in_maps = []
    for i in range(8):
        m = dict(ws)
        m["x"] = x[2 * i:2 * i + 2]
        in_maps.append(m)
    res = run_bass_kernel_spmd(nc, in_maps, core_ids=list(range(8)))
    return np.concatenate([np.asarray(r["out"]) for r in res.results], axis=0).astype(np.float32)
```

```python
import math
import numpy as np
from contextlib import ExitStack
import concourse.bass as bass
import concourse.mybir as mybir
from concourse.bass_utils import run_bass_kernel_spmd

F32 = mybir.dt.float32
BF16 = mybir.dt.bfloat16
U8 = mybir.dt.uint8
AF = mybir.ActivationFunctionType
ALU = mybir.AluOpType

ENGS = ("pe", "act", "dve", "pool", "sp")
EPOCH = 30000
DEFER = False
PRUNE = "pe"
PG = 512

D = 1024
T = 2048
NCHUNK = 512
W = 384
INW = 5376
DFF = 2816
DFE = 3584
NE = 8
LS5 = 128
PI = math.pi


def _dsize(dt):
    return mybir.dt.size(dt)


def keys_of(a):
    if not hasattr(a, "tensor"):
        return [a]
    t = a.tensor
    es = _dsize(a.dtype)
    shp = list(t.shape)
    rb = 1
    for s in shp[1:]:
        rb *= s
    rb *= _dsize(t.dtype)
    ob = a.offset * es
    col0 = ob % rb
    ext = es
    for (step, cnt) in list(a.ap)[1:]:
        ext += (cnt - 1) * abs(step) * es
    p0 = col0 // PG
    p1 = (col0 + ext - 1) // PG
    return [(t.name, p) for p in range(p0, p1 + 1)]


class Prog:
    def __init__(self, nc):
        self.nc = nc
        self.ops = []
        self.last_w = {}
        self.readers = {}
        self.dma_cnt = {}

    def op(self, eng, fn, r=(), w=(), dma=False, sem=None, final=False):
        idx = len(self.ops)
        rk = []
        for a in r:
            rk.extend(keys_of(a))
        wk = []
        for a in w:
            wk.extend(keys_of(a))
        deps = set()
        for k in rk:
            lw = self.last_w.get(k)
            if lw is not None:
                deps.add(lw)
        for k in wk:
            lw = self.last_w.get(k)
            if lw is not None:
                deps.add(lw)
            for rd in self.readers.get(k, ()):
                deps.add(rd)
        deps.discard(idx)
        if eng == "pe":
            deps = {d for d in deps if self.ops[d]["eng"] != "pe"}
        dma_need = {}
        latest = {}
        for d_ in deps:
            od = self.ops[d_]
            if od["dma"]:
                dma_need["d_" + od["sem"]] = 16 * self.dma_cnt[od["sem"]]
            else:
                if latest.get(od["eng"], -1) < d_:
                    latest[od["eng"]] = d_
        if PRUNE == "pe":
            deps = {d_ for d_ in deps if self.ops[d_]["dma"] or self.ops[d_]["eng"] != "pe"}
            if "pe" in latest:
                deps.add(latest["pe"])
        elif PRUNE == "all":
            deps = {d_ for d_ in deps if self.ops[d_]["dma"]} | set(latest.values())
        rec = dict(eng=eng, fn=fn, deps=sorted(deps), dma=dma, sem=None, cnt=None,
                   signal=False, final=final, dma_need=dma_need)
        if dma:
            assert sem is not None
            c = self.dma_cnt.get(sem, 0) + 1
            self.dma_cnt[sem] = c
            rec["sem"] = sem
            rec["cnt"] = c
            rec["signal"] = True
        self.ops.append(rec)
        for k in rk:
            self.readers.setdefault(k, []).append(idx)
        for k in wk:
            self.last_w[k] = idx
            self.readers[k] = []
        return idx

    def emit(self, stack):
        nc = self.nc
        ops = self.ops
        for o in ops:
            for d in o["deps"]:
                ops[d]["signal"] = True
        eng_sig = {e: 0 for e in ENGS}
        for o in ops:
            if o["dma"]:
                continue
            if o["signal"]:
                eng_sig[o["eng"]] += 1
                o["cnt"] = eng_sig[o["eng"]]
        sems = {}

        def getsem(name):
            if name not in sems:
                sems[name] = stack.enter_context(nc.semaphore(name))
            return sems[name]

        for e in ENGS:
            for ep in range(eng_sig[e] // EPOCH + 1):
                getsem(f"e_{e}_{ep}")
        for name in self.dma_cnt:
            getsem("d_" + name)

        def token(o):
            if o["dma"]:
                return ("d_" + o["sem"], 16 * o["cnt"])
            c = o["cnt"]
            ep = (c - 1) // EPOCH
            return (f"e_{o['eng']}_{ep}", c - ep * EPOCH)

        streams = {e: [] for e in ENGS}
        for i, o in enumerate(ops):
            streams[o["eng"]].append(i)
        block = stack.enter_context(nc.Block())

        def run_stream(e, engobj):
            waited = {}
            for i in streams[e]:
                o = ops[i]
                need = {}
                for d in o["deps"]:
                    if ops[d]["dma"]:
                        continue
                    sn, v = token(ops[d])
                    if need.get(sn, 0) < v:
                        need[sn] = v
                for sn, v in o["dma_need"].items():
                    if need.get(sn, 0) < v:
                        need[sn] = v
                for sn, v in need.items():
                    if waited.get(sn, 0) >= v:
                        continue
                    engobj.wait_ge(sems[sn], v)
                    waited[sn] = v
                ins = o["fn"](engobj)
                if o["signal"]:
                    sn, v = token(o)
                    ins.then_inc(sems[sn], 16 if o["dma"] else 1)
            for i in streams[e]:
                o = ops[i]
                if o["dma"] and o["final"]:
                    sn, v = token(o)
                    if waited.get(sn, 0) < v:
                        engobj.wait_ge(sems[sn], v)
                        waited[sn] = v

        @block.tensor
        def _(eng):
            run_stream("pe", eng)

        @block.scalar
        def _(eng):
            run_stream("act", eng)

        @block.vector
        def _(eng):
            run_stream("dve", eng)

        @block.gpsimd
        def _(eng):
            run_stream("pool", eng)

        @block.sync
        def _(eng):
            run_stream("sp", eng)


class Arena:
    def __init__(self, nc, stack, nbytes):
        self.t = stack.enter_context(nc.sbuf_tensor("arena", [128, nbytes], U8))
        self.n = nbytes
        self.top = 0
        self.peak = 0

    def alloc(self, shape, dt=F32, align=PG):
        n = _dsize(dt)
        for s in shape:
            n *= s
        off = (self.top + align - 1) // align * align
        assert off + n <= self.n, f"arena overflow: need {off + n} have {self.n}"
        self.top = off + n
        self.peak = max(self.peak, self.top)
        v = self.t[:, off:off + n].bitcast(dt)
        if len(shape) == 2:
            v = v.rearrange("p (a b) -> p a b", a=shape[0])
        elif len(shape) == 3:
            v = v.rearrange("p (a b c) -> p a b c", a=shape[0], b=shape[1])
        return v

    def mark(self):
        return self.top

    def release(self, m):
        self.top = m


WSPEC = {}


def build(NSEQ=2, NCH=4, DEPTH=2, dbg=(), skip_conv=False):
    nc = bass.Bass("TRN2", target_bir_lowering=False)
    P = Prog(nc)
    st = ExitStack()
    dbg_out = {}

    def din(name, shape):
        return nc.dram_tensor(name, list(shape), F32, kind="ExternalInput").ap()

    x = din("x", [NSEQ, T, D])
    mix_norm_g = din("mix_norm_g", [2, D])
    w_in = din("w_in", [2, D, INW])
    gate_b = din("gate_b", [2, 3 * D])
    s5_lambda_re = din("s5_lambda_re", [2, 24, 64])
    s5_lambda_im = din("s5_lambda_im", [2, 24, 64])
    s5_log_dt = din("s5_log_dt", [2, 24])
    s5_b_re = din("s5_b_re", [2, 24, 64, 16])
    s5_b_im = din("s5_b_im", [2, 24, 64, 16])
    s5_c_re = din("s5_c_re", [2, 24, 16, 64])
    s5_c_im = din("s5_c_im", [2, 24, 16, 64])
    s5_d = din("s5_d", [2, W])
    s5_w_glu = din("s5_w_glu", [2, W, W])
    s5_b_glu = din("s5_b_glu", [2, W])
    conv_w = din("conv_w", [2, 4, W])
    conv_b = din("conv_b", [2, W])
    lru_w_a = din("lru_w_a", [2, 6, 64, 64])
    lru_b_a = din("lru_b_a", [2, W])
    lru_w_x = din("lru_w_x", [2, 6, 64, 64])
    lru_b_x = din("lru_b_x", [2, W])
    lru_lambda = din("lru_lambda", [2, W])
    w_branch = din("w_branch", [2, 3, W, D])
    w_out = din("w_out", [2, D, D])
    ffn_norm_g = din("ffn_norm_g", [2, D])
    ffn_w_gate = din("ffn_w_gate", [1, D, DFF])
    ffn_w_up = din("ffn_w_up", [1, D, DFF])
    ffn_w_down = din("ffn_w_down", [1, DFF, D])
    router_w = din("router_w", [1, D, NE])
    router_b = din("router_b", [1, NE])
    moe_w_gate = din("moe_w_gate", [1, NE, D, DFE])
    moe_w_up = din("moe_w_up", [1, NE, D, DFE])
    moe_w_down = din("moe_w_down", [1, NE, DFE, D])
    final_norm_g = din("final_norm_g", [D])
    out = nc.dram_tensor("out", [NSEQ, T, D], F32, kind="ExternalOutput").ap()

    A = Arena(nc, st, 200704)
    psb = [st.enter_context(nc.psum_tensor(f"ps{i}", [128, 512], F32)).ap() for i in range(8)]
    ps_rr = [0]

    def nb():
        i = ps_rr[0]
        ps_rr[0] = (i + 1) % 8
        return psb[i]

    def dump(name, ap2d, shape):
        if name not in dbg:
            return
        o = nc.dram_tensor("dbg_" + name, list(shape), ap2d.dtype, kind="ExternalOutput").ap()
        dbg_out[name] = o
        P.op("sp", lambda e: e.dma_start(out=o, in_=ap2d), r=[ap2d], w=["dbg_" + name],
             dma=True, sem="dbg_" + name, final=True)

    def act(out_, in_, func, r=None, **kw):
        rr = [in_] + [v for v in kw.values() if hasattr(v, "tensor")]
        P.op("act", lambda e: e.activation(out=out_, in_=in_, func=func, **kw), r=rr, w=[out_])

    def tt(eng, out_, a, b, op):
        P.op(eng, lambda e: e.tensor_tensor(out=out_, in0=a, in1=b, op=op), r=[a, b], w=[out_])

    def ts(eng, out_, a, s1, op0, s2=None, op1=None):
        rr = [a] + [s for s in (s1, s2) if hasattr(s, "tensor")]
        if op1 is None:
            P.op(eng, lambda e: e.tensor_scalar(out=out_, in0=a, scalar1=s1, scalar2=None, op0=op0), r=rr, w=[out_])
        else:
            P.op(eng, lambda e: e.tensor_scalar(out=out_, in0=a, scalar1=s1, scalar2=s2, op0=op0, op1=op1), r=rr, w=[out_])

    def stt(out_, a, s, b, op0, op1):
        rr = [a, b] + ([s] if hasattr(s, "tensor") else [])
        P.op("dve", lambda e: e.scalar_tensor_tensor(out=out_, in0=a, scalar=s, in1=b, op0=op0, op1=op1), r=rr, w=[out_])

    def cp(eng, out_, in_):
        if eng == "act":
            P.op("act", lambda e: e.activation(out=out_, in_=in_, func=AF.Copy), r=[in_], w=[out_])
        else:
            P.op(eng, lambda e: e.tensor_copy(out=out_, in_=in_), r=[in_], w=[out_])

    def memset(eng, ap, val):
        P.op(eng, lambda e: e.memset(ap, val), w=[ap])

    def mm(out_, lhsT, rhs, start, stop):
        P.op("pe", lambda e: e.matmul(out_, lhsT=lhsT, rhs=rhs, start=start, stop=stop), r=[lhsT, rhs], w=[out_])

    def tr(out_, in_, ident):
        P.op("pe", lambda e: e.transpose(out=out_, in_=in_, identity=ident), r=[in_, ident], w=[out_])

    def dma(out_, in_, sem, r=(), w=(), final=False, slow=False):
        rr = list(r) + ([in_] if in_.tensor.name == "arena" else [])
        ww = list(w) + ([out_] if out_.tensor.name == "arena" else [])
        if slow:
            P.op("sp", lambda e: e.dma_start(out=out_, in_=in_, allow_slow_non_contiguous=True), r=rr, w=ww, dma=True, sem=sem, final=final)
        else:
            P.op("sp", lambda e: e.dma_start(out=out_, in_=in_), r=rr, w=ww, dma=True, sem=sem, final=final)

    ident_f = A.alloc([128], F32)
    ones_f = A.alloc([128], F32)
    ident_b = A.alloc([128], BF16, align=256)
    ones_b = A.alloc([128], BF16, align=256)
    trineg = A.alloc([128], BF16, align=256)
    masks = A.alloc([4, 512], BF16)
    tmpc = A.alloc([512], F32)
    memset("pool", ones_f, 1.0)
    P.op("pool", lambda e: e.affine_select(out=ident_f, in_=ones_f, pattern=[[-1, 128]], compare_op=ALU.is_equal,
                                           fill=0.0, base=0, channel_multiplier=1), r=[ones_f], w=[ident_f])
    cp("pool", ident_b, ident_f)
    cp("pool", ones_b, ones_f)
    memset("pool", tmpc, -1.0)
    P.op("pool", lambda e: e.affine_select(out=tmpc[:, 0:128], in_=tmpc[:, 0:128], pattern=[[-1, 128]], compare_op=ALU.is_ge,
                                           fill=0.0, base=0, channel_multiplier=1), r=[tmpc], w=[tmpc])
    cp("pool", trineg, tmpc[:, 0:128])
    for r_ in range(4):
        memset("pool", tmpc, 1.0)
        P.op("pool", lambda e, r_=r_: e.affine_select(out=tmpc, in_=tmpc, pattern=[[1, 512]], compare_op=ALU.is_gt,
                                                     fill=0.0, base=-128 * r_, channel_multiplier=-1), r=[tmpc], w=[tmpc])
        cp("pool", masks[:, r_, :], tmpc)

    rows = {}
    nrow = [0, 0]
    stage = [A.alloc([128], F32), A.alloc([128], F32)]
    memset("dve", stage[0], 0.0)
    memset("dve", stage[1], 0.0)
    ptab = [A.alloc([128], F32), A.alloc([128], F32)]
    pcount = [0]

    def addvec(name, l, src2d, n, si):
        r0 = nrow[si]
        nrow[si] += n
        assert nrow[si] <= 128
        rows[(name, l)] = (si, r0)
        pcount[0] += 1
        dma(stage[si][r0:r0 + n, :], src2d, sem="pstage", r=["in_" + name])

    ldt_s = A.alloc([2, 2], F32, align=64)
    for l in range(2):
        nrow[l] = 12
        rows[("log_dt", l)] = (l, 0)
        dma(ldt_s[0:12, l, :], s5_log_dt[l].rearrange("(m two) -> m two", two=2), sem=f"pldt{l}")
        cp("dve", stage[l][0:12, :].rearrange("m (two q) -> m two q", two=2),
           ldt_s[0:12, l, :].unsqueeze(2).to_broadcast([12, 2, 64]))
    for l in range(2):
        addvec("mix_g", l, mix_norm_g[l].rearrange("(r c) -> r c", c=128), 8, l)
        addvec("ffn_g", l, ffn_norm_g[l].rearrange("(r c) -> r c", c=128), 8, l)
        addvec("gate_b", l, gate_b[l].rearrange("(r c) -> r c", c=128), 24, l)
        addvec("s5_d", l, s5_d[l].rearrange("(r c) -> r c", c=128), 3, l)
        addvec("b_glu", l, s5_b_glu[l].rearrange("(r c) -> r c", c=128), 3, l)
        addvec("conv_w", l, conv_w[l].rearrange("k (r c) -> (k r) c", c=128), 12, l)
        addvec("conv_b", l, conv_b[l].rearrange("(r c) -> r c", c=128), 3, l)
        addvec("b_a", l, lru_b_a[l].rearrange("(r c) -> r c", c=128), 3, l)
        addvec("b_x", l, lru_b_x[l].rearrange("(r c) -> r c", c=128), 3, l)
        addvec("lam", l, lru_lambda[l].rearrange("(r c) -> r c", c=128), 3, l)
        addvec("lam_re", l, s5_lambda_re[l].rearrange("(m two) q -> m (two q)", two=2), 12, l)
        addvec("lam_im", l, s5_lambda_im[l].rearrange("(m two) q -> m (two q)", two=2), 12, l)
    addvec("fin_g", 0, final_norm_g.rearrange("(r c) -> r c", c=128), 8, 0)
    for si in range(2):
        pb = nb()
        tr(pb[:, 0:128], stage[si], ident_f)
        cp("dve", ptab[si], pb[:, 0:128])

    def pcol(name, l, j=0, n=1):
        si, r0 = rows[(name, l)]
        return ptab[si][:, r0 + j:r0 + j + n]

    rw = A.alloc([8, 8], F32, align=64)
    rb_bc = A.alloc([8], F32, align=64)
    dma(rw, router_w[0].rearrange("(kt p) e -> p kt e", p=128), sem="pstage2")
    dma(rb_bc, router_b[0:1, :].to_broadcast([128, 8]), sem="pstage2")

    scr = {}

    def declare_w(name, K, N, cb):
        kt = K // 128
        nt = N // cb
        t = nc.dram_tensor("scr_" + name, [nt, 128, kt, cb], BF16, kind="Internal").ap()
        scr[name] = (t, kt, nt, cb)

    conv_jobs = []
    for l in range(2):
        declare_w(f"win{l}", D, INW, 384)
        conv_jobs.append((f"win{l}", w_in[l], l))
        for n in range(3):
            declare_w(f"wbr{l}_{n}", W, D, 256)
            conv_jobs.append((f"wbr{l}_{n}", w_branch[l, n], l))
        declare_w(f"wout{l}", D, D, 256)
        conv_jobs.append((f"wout{l}", w_out[l], l))
        declare_w(f"glu{l}", W, W, 384)
        conv_jobs.append((f"glu{l}", s5_w_glu[l], l))
    declare_w("fg", D, DFF, 256)
    declare_w("fu", D, DFF, 256)
    declare_w("fd", DFF, D, 128)
    conv_jobs += [("fg", ffn_w_gate[0], 0), ("fu", ffn_w_up[0], 0), ("fd", ffn_w_down[0], 0)]
    for e_ in range(NE):
        declare_w(f"mg{e_}", D, DFE, 256)
        declare_w(f"mu{e_}", D, DFE, 256)
        declare_w(f"md{e_}", DFE, D, 128)
        conv_jobs += [(f"mg{e_}", moe_w_gate[0, e_], 1), (f"mu{e_}", moe_w_up[0, e_], 1), (f"md{e_}", moe_w_down[0, e_], 1)]
    xmid = nc.dram_tensor("xmid", [NSEQ, NCH, 128, 8, NCHUNK], F32, kind="Internal").ap()

    if not skip_conv:
        mk = A.mark()
        NST = 8
        stg = [A.alloc([2048], F32) for _ in range(NST)]
        stb = [A.alloc([2048], BF16) for _ in range(NST)]
        step = 0
        cast_engs = ["dve", "act"]
        pend = []

        def finish(job):
            (i, st_, t, kti, c0, cw, cb, name) = job
            cp(cast_engs[st_ % 2], stb[i][:, 0:cw], stg[i][:, 0:cw])
            t0 = c0 // cb
            ntl = cw // cb
            dma(t[t0:t0 + ntl, :, kti, :].rearrange("t p c -> p t c"),
                stb[i][:, 0:cw].rearrange("p (t c) -> p t c", c=cb), sem=f"stb{i}",
                w=[("scr", name, tt_, kti) for tt_ in range(t0, t0 + ntl)])

        for (name, src, lyr) in conv_jobs:
            if lyr >= DEPTH or (DEFER and lyr >= 1):
                continue
            t, kt, nt, cb = scr[name]
            N = nt * cb
            CW = (2048 // cb) * cb
            for kti in range(kt):
                for c0 in range(0, N, CW):
                    cw = min(CW, N - c0)
                    i = step % NST
                    if len(pend) >= NST - 1:
                        finish(pend.pop(0))
                    dma(stg[i][:, 0:cw], src[kti * 128:(kti + 1) * 128, c0:c0 + cw], sem=f"stg{i}")
                    pend.append((i, step, t, kti, c0, cw, cb, name))
                    step += 1
        while pend:
            finish(pend.pop(0))
        A.release(mk)

    NSTB = 4
    dstg = [A.alloc([1024], F32) for _ in range(NSTB)] if DEFER else []
    dstb = [A.alloc([1024], BF16) for _ in range(NSTB)] if DEFER else []
    dsteps = []
    if DEFER and not skip_conv and DEPTH > 1:
        for (name, src, lyr) in conv_jobs:
            if lyr < 1:
                continue
            t, kt, nt, cb = scr[name]
            N = nt * cb
            CW = (1024 // cb) * cb
            for kti in range(kt):
                for c0 in range(0, N, CW):
                    dsteps.append((name, src, t, kti, c0, min(CW, N - c0), cb))
    dstate = dict(next=0, pend=[])

    def dfinish(job):
        (i, k, name, src, t, kti, c0, cw, cb) = job
        cp("dve" if k % 2 else "act", dstb[i][:, 0:cw], dstg[i][:, 0:cw])
        t0 = c0 // cb
        ntl = cw // cb
        dma(t[t0:t0 + ntl, :, kti, :].rearrange("t p c -> p t c"),
            dstb[i][:, 0:cw].rearrange("p (t c) -> p t c", c=cb), sem=f"dstb{i}",
            w=[("scr", name, tt_, kti) for tt_ in range(t0, t0 + ntl)])

    def emit_conv(nsteps, flush=False):
        for _ in range(nsteps):
            k = dstate["next"]
            if k >= len(dsteps):
                break
            dstate["next"] = k + 1
            (name, src, t, kti, c0, cw, cb) = dsteps[k]
            i = k % NSTB
            if len(dstate["pend"]) >= NSTB - 1:
                dfinish(dstate["pend"].pop(0))
            dma(dstg[i][:, 0:cw], src[kti * 128:(kti + 1) * 128, c0:c0 + cw], sem=f"dstg{i}")
            dstate["pend"].append((i, k, name, src, t, kti, c0, cw, cb))
        if flush:
            while dstate["pend"]:
                dfinish(dstate["pend"].pop(0))

    rings = {}

    def wring(cls, shape, nbuf):
        rings[cls] = dict(bufs=[A.alloc(shape, BF16) for _ in range(nbuf)], i=0)

    def wload(cls, name, ti, slot=None):
        rg = rings[cls]
        if slot is None:
            i = rg["i"]
            rg["i"] = (i + 1) % len(rg["bufs"])
        else:
            i = slot
        buf = rg["bufs"][i]
        t, kt, nt, cb = scr[name]
        dst = buf[:, 0:kt, 0:cb]
        dma(dst, t[ti], sem=f"w_{cls}_{i}", r=[("scr", name, ti, k) for k in range(kt)])
        return dst

    c = {}
    c["Ere"] = A.alloc([12, LS5], BF16)
    c["Eim"] = A.alloc([12, LS5], BF16)
    c["Bre"] = A.alloc([12, 128], BF16)
    c["Bim"] = A.alloc([12, 128], BF16)
    c["Cre"] = A.alloc([12, 128], BF16)
    c["Cimn"] = A.alloc([12, 128], BF16)
    c["Dd"] = A.alloc([3, 128], BF16, align=256)
    c["r"] = A.alloc([12], F32, align=64)
    c["rtab"] = A.alloc([12, LS5], F32)
    c["Wa"] = A.alloc([3, 128], F32)
    c["Wx"] = A.alloc([3, 128], F32)
    c["clru"] = A.alloc([3], F32, align=64)
    c["kT"] = A.alloc([3, T], BF16)
    c["vc"] = A.alloc([16, W], BF16)
    c["slre"] = A.alloc([12], F32, align=64)
    c["slim"] = A.alloc([12], F32, align=64)
    c["hst"] = A.alloc([3], F32, align=64)
    c["halo"] = A.alloc([3, 4], F32, align=64)

    negones = A.alloc([128], F32)
    memset("pool", negones, -1.0)

    xTs = [A.alloc([8, NCHUNK], F32)]
    hn = A.alloc([8, NCHUNK], BF16)
    GELU = AF.Gelu_apprx_tanh

    def prep_layer(l):
        mk = A.mark()
        sm = lambda: A.alloc([12], F32, align=64)
        lr = pcol("lam_re", l, 0, 12)
        li = pcol("lam_im", l, 0, 12)
        ldt = pcol("log_dt", l, 0, 12)
        dt = sm(); lrd = sm(); th = sm(); ea = c["r"]; tq = sm(); th2 = sm()
        sn = sm(); cs = sm(); are = sm(); aim = sm(); den = sm(); rden = sm(); nr = sm()
        cre = sm(); cim = sm(); t1 = sm(); t2 = sm()
        act(dt, ldt, AF.Exp)
        tt("dve", lrd, lr, dt, ALU.mult)
        tt("dve", th, li, dt, ALU.mult)
        act(ea, lrd, AF.Exp)
        for Pp in (64 * PI, 32 * PI, 16 * PI, 8 * PI, 4 * PI, 2 * PI):
            ts("dve", tq, th, Pp - PI, ALU.is_ge, Pp, ALU.mult)
            tt("dve", th, th, tq, ALU.subtract)
        ts("dve", th2, th, PI / 2, ALU.add)
        ts("dve", tq, th2, PI, ALU.is_ge, 2 * PI, ALU.mult)
        tt("dve", th2, th2, tq, ALU.subtract)
        act(sn, th, AF.Sin)
        act(cs, th2, AF.Sin)
        tt("dve", are, ea, cs, ALU.mult)
        tt("dve", aim, ea, sn, ALU.mult)
        tt("dve", t1, lr, lr, ALU.mult)
        tt("dve", t2, li, li, ALU.mult)
        tt("dve", den, t1, t2, ALU.add)
        P.op("dve", lambda e, rden=rden, den=den: e.reciprocal(out=rden, in_=den), r=[den], w=[rden])
        ts("dve", nr, are, -1.0, ALU.add)
        tt("dve", t1, nr, lr, ALU.mult)
        tt("dve", t2, aim, li, ALU.mult)
        tt("dve", t1, t1, t2, ALU.add)
        tt("dve", cre, t1, rden, ALU.mult)
        tt("dve", t1, aim, lr, ALU.mult)
        tt("dve", t2, nr, li, ALU.mult)
        tt("dve", t1, t1, t2, ALU.subtract)
        tt("dve", cim, t1, rden, ALU.mult)
        Ere = A.alloc([12, LS5], F32)
        Eim = A.alloc([12, LS5], F32)
        cp("dve", Ere[:, :, 0:1], cs.unsqueeze(2))
        cp("dve", Eim[:, :, 0:1], sn.unsqueeze(2))
        ta = A.alloc([12, 64], F32)
        tb = A.alloc([12, 64], F32)
        k = 1
        while k < LS5:
            ck = Ere[:, :, k - 1:k].to_broadcast([128, 12, k])
            sk = Eim[:, :, k - 1:k].to_broadcast([128, 12, k])
            tt("dve", ta[:, :, 0:k], Ere[:, :, 0:k], ck, ALU.mult)
            tt("dve", tb[:, :, 0:k], Eim[:, :, 0:k], sk, ALU.mult)
            tt("dve", Ere[:, :, k:2 * k], ta[:, :, 0:k], tb[:, :, 0:k], ALU.subtract)
            tt("dve", ta[:, :, 0:k], Ere[:, :, 0:k], sk, ALU.mult)
            tt("dve", tb[:, :, 0:k], Eim[:, :, 0:k], ck, ALU.mult)
            tt("dve", Eim[:, :, k:2 * k], ta[:, :, 0:k], tb[:, :, 0:k], ALU.add)
            k *= 2
        memset("pool", c["rtab"], 0.0)
        cp("pool", c["rtab"][:, :, 1:LS5], c["r"].unsqueeze(2).to_broadcast([128, 12, LS5 - 1]))
        cp("dve", c["Ere"], Ere)
        cp("dve", c["Eim"], Eim)
        if l == 0:
            dump("Ere", Ere.rearrange("p a b -> p (a b)"), [128, 12 * LS5])
            dump("Eim", Eim.rearrange("p a b -> p (a b)"), [128, 12 * LS5])
            dump("r", c["r"], [128, 12])
            dump("cre", cre, [128, 12])
            dump("cim", cim, [128, 12])
        bre = A.alloc([12, 16], F32)
        bim = A.alloc([12, 16], F32)
        dma(bre, s5_b_re[l].rearrange("(m two) q h -> (two q) m h", two=2), sem="prep")
        dma(bim, s5_b_im[l].rearrange("(m two) q h -> (two q) m h", two=2), sem="prep")
        Bbr = A.alloc([12, 16], F32)
        Bbi = A.alloc([12, 16], F32)
        u1 = A.alloc([12, 16], F32)
        u2 = A.alloc([12, 16], F32)
        creb = cre.unsqueeze(2).to_broadcast([128, 12, 16])
        cimb = cim.unsqueeze(2).to_broadcast([128, 12, 16])
        tt("dve", u1, bre, creb, ALU.mult)
        tt("dve", u2, bim, cimb, ALU.mult)
        tt("dve", Bbr, u1, u2, ALU.subtract)
        tt("dve", u1, bim, creb, ALU.mult)
        tt("dve", u2, bre, cimb, ALU.mult)
        tt("dve", Bbi, u1, u2, ALU.add)
        X = A.alloc([12, 128], F32)
        for (src_, dstname) in ((Bbr, "Bre"), (Bbi, "Bim")):
            memset("pool", X, 0.0)
            for m in range(12):
                for two in range(2):
                    g = 2 * m + two
                    c0 = 16 * (g % 8)
                    cp("pool", X[two * 64:(two + 1) * 64, m, c0:c0 + 16], src_[two * 64:(two + 1) * 64, m, :])
            for m4 in range(3):
                pb = nb()
                for mi in range(4):
                    tr(pb[:, mi * 128:(mi + 1) * 128], X[:, m4 * 4 + mi, :], ident_f)
                cp("dve", c[dstname][:, m4 * 4:(m4 + 1) * 4, :], pb.rearrange("p (a b) -> p a b", a=4))
        for (srcC, dstname, scale) in ((s5_c_re, "Cre", 1.0), (s5_c_im, "Cimn", -1.0)):
            memset("pool", c[dstname], 0.0)
            for yt in range(3):
                ct = X[:, yt, :]
                srcv = srcC[l].rearrange("(y g) h q -> y (g h) q", g=8)[yt]
                dma(ct[:, 0:64], srcv, sem="prep")
                dma(ct[:, 64:128], srcv, sem="prep")
                pb = nb()
                tr(pb[:, 0:128], ct, ident_f)
                for mi in range(4):
                    m = 4 * yt + mi
                    for two in range(2):
                        g = 2 * m + two
                        c0 = 16 * (g % 8)
                        ts("dve", c[dstname][two * 64:(two + 1) * 64, m, c0:c0 + 16],
                           pb[two * 64:(two + 1) * 64, c0:c0 + 16], scale, ALU.mult)
        for yt in range(3):
            ts("dve", c["Dd"][:, yt, :], ident_f, pcol("s5_d", l, yt), ALU.mult)
        memset("pool", c["Wa"], 0.0)
        memset("pool", c["Wx"], 0.0)
        for kt in range(3):
            for half in range(2):
                hsl = slice(half * 64, (half + 1) * 64)
                dma(c["Wa"][hsl, kt, half * 64:(half + 1) * 64], lru_w_a[l, 2 * kt + half], sem="prep")
                dma(c["Wx"][hsl, kt, half * 64:(half + 1) * 64], lru_w_x[l, 2 * kt + half], sem="prep")
        t3 = A.alloc([3], F32, align=64)
        act(t3, pcol("lam", l, 0, 3), AF.Exp, scale=-1.0)
        act(t3, t3, AF.Ln, bias=1.0)
        ts("dve", c["clru"], t3, -8.0, ALU.mult)
        A.release(mk)

    def rmsnorm(xT, gname, l, out_bf=None, out_f=None):
        mk = A.mark()
        sq = [A.alloc([NCHUNK], BF16), A.alloc([NCHUNK], BF16)]
        std = A.alloc([NCHUNK], F32)
        rstd = A.alloc([NCHUNK], F32)
        pb = nb()
        for dk in range(8):
            act(sq[dk % 2], xT[:, dk, :], AF.Square)
            mm(pb, ones_b, sq[dk % 2], dk == 0, dk == 7)
        act(std, pb, AF.Sqrt, scale=1.0 / D, bias=1e-6)
        P.op("dve", lambda e: e.reciprocal(out=rstd, in_=std), r=[std], w=[rstd])
        for dk in range(8):
            if out_bf is not None:
                stt(out_bf[:, dk, :], xT[:, dk, :], pcol(gname, l, dk), rstd, ALU.mult, ALU.mult)
            if out_f is not None:
                stt(out_f[:, dk, :], xT[:, dk, :], pcol(gname, l, dk), rstd, ALU.mult, ALU.mult)
        A.release(mk)

    def mixer(l, b, c_, xT, first):
        t0 = c_ * NCHUNK
        mkL = A.mark()
        wring("win", [8, 384], 3)
        wring("wbr", [3, 256], 3)
        wring("wout", [8, 256], 2)
        wring("glu", [3, 384], 1)
        rmsnorm(xT, "mix_g", l, out_bf=hn)
        if first:
            dump("hn", hn.rearrange("p a b -> p (a b)"), [128, 8 * NCHUNK])
        uT = A.alloc([3, NCHUNK], BF16)
        qT = A.alloc([3, NCHUNK], BF16)
        xl = A.alloc([3, NCHUNK + 4], F32)
        gy = A.alloc([3, NCHUNK], BF16)
        a1 = A.alloc([3, NCHUNK], BF16)
        aT = A.alloc([3, NCHUNK], BF16)
        bT = A.alloc([3, NCHUNK], BF16)
        cT = A.alloc([3, NCHUNK], BF16)
        kT, vc = c["kT"], c["vc"]
        if c_ == 0:
            memset("pool", xl[:, :, 0:3], 0.0)
        else:
            cp("pool", xl[:, :, 0:3], c["halo"][:, :, 0:3])
        for wt in range(6):
            wtile = wload("win", f"win{l}", wt)
            if wt == 3:
                for tt_ in range(4):
                    pb = nb()
                    for dk in range(8):
                        mm(pb[:, 0:W], hn[:, dk, tt_ * 128:(tt_ + 1) * 128], wtile[:, dk, :], dk == 0, dk == 7)
                    cp("act" if tt_ % 2 else "dve", vc[:, 4 * c_ + tt_, :], pb[:, 0:W])
                continue
            for j in range(3):
                pb = nb()
                for dk in range(8):
                    mm(pb, wtile[:, dk, j * 128:(j + 1) * 128], hn[:, dk, :], dk == 0, dk == 7)
                if wt == 0:
                    cp("act", uT[:, j, :], pb)
                elif wt == 1:
                    act(qT[:, j, :], pb, AF.Copy, scale=0.125)
                elif wt == 2:
                    cp("dve", kT[:, j, t0:t0 + NCHUNK], pb)
                elif wt == 4:
                    cp("dve", xl[:, j, 3:3 + NCHUNK], pb)
                elif wt == 5:
                    act(gy[:, j, :], pb, GELU)
        cp("pool", c["halo"][:, :, 0:3], xl[:, :, NCHUNK:NCHUNK + 3])
        if l == 0:
            emit_conv(CONV_SLICE)
        if first:
            dump("uT", uT.rearrange("p a b -> p (a b)"), [128, 3 * NCHUNK])
            dump("qT", qT.rearrange("p a b -> p (a b)"), [128, 3 * NCHUNK])
            dump("vc", vc[:, 0:4, :].rearrange("p a b -> p (a b)"), [128, 4 * W])
        mk = A.mark()
        NSET = 3
        S5T = [[A.alloc([4, LS5], F32) for _ in range(4)] for _ in range(NSET)]
        S5B = [[A.alloc([4, LS5], BF16) for _ in range(2)] for _ in range(NSET)]
        rin = [[A.alloc([4], F32, align=64) for _ in range(2)] for _ in range(NSET)]
        fl = lambda v: v.rearrange("p a b -> p (a b)")
        xc = A.alloc([NCHUNK], F32); av = A.alloc([NCHUNK], F32); ig = A.alloc([NCHUNK], F32)
        om = A.alloc([NCHUNK], F32); hh = A.alloc([NCHUNK], F32)

        s5_R = [psb[3], psb[5]]
        s5_I = [psb[4], psb[6]]
        s5_Y = psb[7]

        def s5_A(it, sc, yt):
            o = sc * LS5
            b1, b2, b3, b4 = S5T[it % NSET]
            rn = rin[it % NSET]
            m0 = 4 * yt
            R = s5_R[it % 2].rearrange("p (a b) -> p a b", a=4)
            I = s5_I[it % 2].rearrange("p (a b) -> p a b", a=4)
            for mi in range(4):
                mm(R[:, mi, :], c["Bre"][:, m0 + mi, :], uT[:, yt, o:o + LS5], True, True)
                mm(I[:, mi, :], c["Bim"][:, m0 + mi, :], uT[:, yt, o:o + LS5], True, True)
            Er4 = c["Ere"][:, m0:m0 + 4, :]
            Ei4 = c["Eim"][:, m0:m0 + 4, :]
            tt("dve", b1, R, Er4, ALU.mult)
            tt("dve", b2, I, Ei4, ALU.mult)
            tt("dve", b3, I, Er4, ALU.mult)
            tt("dve", b4, R, Ei4, ALU.mult)
            tt("dve", b1, b1, b2, ALU.add)
            tt("pool", b3, b3, b4, ALU.subtract)
            if not (c_ == 0 and sc == 0):
                tt("dve", rn[0], c["r"][:, m0:m0 + 4], c["slre"][:, m0:m0 + 4], ALU.mult)
                tt("dve", rn[1], c["r"][:, m0:m0 + 4], c["slim"][:, m0:m0 + 4], ALU.mult)
                tt("dve", b1[:, :, 0:1], b1[:, :, 0:1], rn[0].unsqueeze(2), ALU.add)
                tt("dve", b3[:, :, 0:1], b3[:, :, 0:1], rn[1].unsqueeze(2), ALU.add)

        def s5_B(it, sc, yt):
            b1, b2, b3, b4 = S5T[it % NSET]
            sbf = S5B[it % NSET]
            m0 = 4 * yt
            Er4 = c["Ere"][:, m0:m0 + 4, :]
            Ei4 = c["Eim"][:, m0:m0 + 4, :]
            rt4 = c["rtab"][:, m0:m0 + 4, :]
            for (z_, w_) in ((b2, b1), (b4, b3)):
                P.op("dve", lambda e, z_=z_, w_=w_, rt4=rt4: e.tensor_tensor_scan(
                    out=fl(z_), data0=fl(rt4), data1=fl(w_), initial=0.0, op0=ALU.mult, op1=ALU.add),
                    r=[w_, rt4], w=[z_])
            tt("dve", b1, b2, Er4, ALU.mult)
            tt("pool", b3, b4, Ei4, ALU.mult)
            tt("pool", sbf[0], b1, b3, ALU.subtract)
            tt("dve", c["slre"][:, m0:m0 + 4].unsqueeze(2), b1[:, :, LS5 - 1:LS5], b3[:, :, LS5 - 1:LS5], ALU.subtract)
            tt("pool", b1, b4, Er4, ALU.mult)
            tt("pool", b3, b2, Ei4, ALU.mult)
            tt("pool", sbf[1], b1, b3, ALU.add)
            tt("dve", c["slim"][:, m0:m0 + 4].unsqueeze(2), b1[:, :, LS5 - 1:LS5], b3[:, :, LS5 - 1:LS5], ALU.add)

        def s5_C(it, sc, yt):
            o = sc * LS5
            sbf = S5B[it % NSET]
            m0 = 4 * yt
            q = it % 4
            Y = s5_Y[:, q * LS5:(q + 1) * LS5]
            mm(Y, c["Dd"][:, yt, :], uT[:, yt, o:o + LS5], True, False)
            for mi in range(4):
                mm(Y, c["Cre"][:, m0 + mi, :], sbf[0][:, mi, :], False, False)
                mm(Y, c["Cimn"][:, m0 + mi, :], sbf[1][:, mi, :], False, mi == 3)
            act(a1[:, yt, o:o + LS5], Y, GELU)

        def lru_step(kt):
            ts("dve", xc, xl[:, kt, 0:NCHUNK], pcol("conv_w", l, 0 * 3 + kt), ALU.mult, pcol("conv_b", l, kt), ALU.add)
            for k in range(1, 4):
                stt(xc, xl[:, kt, k:k + NCHUNK], pcol("conv_w", l, k * 3 + kt), xc, ALU.mult, ALU.add)
            pa = psb[1]
            px = psb[2]
            mm(pa, c["Wa"][:, kt, :], xc, True, True)
            mm(px, c["Wx"][:, kt, :], xc, True, True)
            act(av, pa, AF.Sigmoid, bias=pcol("b_a", l, kt))
            act(av, av, AF.Exp, scale=c["clru"][:, kt:kt + 1])
            act(ig, px, AF.Sigmoid, bias=pcol("b_x", l, kt))
            tt("pool", om, av, av, ALU.mult)
            ts("pool", om, om, -1.0, ALU.mult, 1.0, ALU.add)
            act(om, om, AF.Sqrt)
            tt("pool", ig, ig, xc, ALU.mult)
            tt("pool", om, om, ig, ALU.mult)
            if c_ == 0:
                P.op("dve", lambda e: e.tensor_tensor_scan(
                    out=hh, data0=av, data1=om, initial=0.0, op0=ALU.mult, op1=ALU.add), r=[av, om], w=[hh])
            else:
                ini = c["hst"][:, kt:kt + 1]
                P.op("dve", lambda e, ini=ini: e.tensor_tensor_scan(
                    out=hh, data0=av, data1=om, initial=ini, op0=ALU.mult, op1=ALU.add), r=[av, om, ini], w=[hh])
            cp("dve", c["hst"][:, kt:kt + 1], hh[:, NCHUNK - 1:NCHUNK])
            tt("pool", cT[:, kt, :], hh, gy[:, kt, :], ALU.mult)

        steps = [(sc * 3 + yt, sc, yt) for sc in range(NCHUNK // LS5) for yt in range(3)]
        ns = len(steps)
        for k in range(ns + 2):
            if k < ns:
                s5_A(*steps[k])
            if 0 <= k - 1 < ns:
                s5_B(*steps[k - 1])
            if 0 <= k - 2 < ns:
                s5_C(*steps[k - 2])
            if k in (3, 7, 11):
                lru_step(k // 4)
        if l == 0 and DEFER:
            emit_conv(CONV_SLICE)
        wg = wload("glu", f"glu{l}", 0)
        sg = [A.alloc([NCHUNK], BF16), A.alloc([NCHUNK], BF16)]
        for j in range(3):
            pb = nb()
            for kt in range(3):
                mm(pb, wg[:, kt, j * 128:(j + 1) * 128], a1[:, kt, :], kt == 0, kt == 2)
            act(sg[j % 2], pb, AF.Sigmoid, bias=pcol("b_glu", l, j))
            tt("pool", aT[:, j, :], a1[:, j, :], sg[j % 2], ALU.mult)
        A.release(mk)
        if first:
            dump("a1", a1.rearrange("p a b -> p (a b)"), [128, 3 * NCHUNK])
            dump("aT", aT.rearrange("p a b -> p (a b)"), [128, 3 * NCHUNK])
            dump("cT", cT.rearrange("p a b -> p (a b)"), [128, 3 * NCHUNK])
        mk = A.mark()
        NPIPE = 2
        ex_ = [[A.alloc([NCHUNK], BF16) for _ in range(2)] for _ in range(NPIPE)]
        sp_ = ex_
        ar_ = [[A.alloc([NCHUNK], F32) for _ in range(2)] for _ in range(NPIPE)]
        ww_ = [[A.alloc([NCHUNK], BF16) for _ in range(2)] for _ in range(NPIPE)]
        carry = [A.alloc([NCHUNK], F32) for _ in range(2)]
        zb = [[psb[0], psb[1]], [psb[2], psb[3]]]
        ob = psb[4]
        csb = [psb[5], psb[6]]
        items = []
        for hp in range(3):
            for B in range(4 * c_ + 3, -1, -1):
                items.append((hp, B))

        def s1(n):
            hp, B = items[n]
            for hh_ in range(2):
                ps_ = slice(hh_ * 64, (hh_ + 1) * 64)
                mm(zb[n % 2][hh_], kT[ps_, hp, B * 128:(B + 1) * 128], qT[ps_, hp, :], True, False)

        def part1(n):
            hp, B = items[n]
            pi = n % NPIPE
            diag = B >= 4 * c_
            lastB = (B == 0)
            firstB = (B == 4 * c_ + 3)
            for hh_ in range(2):
                z = zb[n % 2][hh_]
                e_ = ex_[pi][hh_]; s_ = sp_[pi][hh_]; a_ = ar_[pi][hh_]
                act(e_, z, AF.Exp)
                act(s_, e_, AF.Ln, bias=1.0)
                if diag:
                    tt("pool", s_, s_, masks[:, B - 4 * c_, :], ALU.mult)
                mm(z, trineg, s_, False, True)
                if firstB:
                    cp("dve", a_, z)
                else:
                    tt("dve", a_, z, carry[hh_], ALU.subtract)
                if not lastB:
                    mm(csb[hh_], ones_b, s_, True, True)
                    if firstB:
                        cp("dve", carry[hh_], csb[hh_])
                    else:
                        tt("dve", carry[hh_], csb[hh_], carry[hh_], ALU.add)

        def part2(n):
            hp, B = items[n]
            pi = n % NPIPE
            diag = B >= 4 * c_
            lastB = (B == 0)
            firstB = (B == 4 * c_ + 3)
            for hh_ in range(2):
                h = 2 * hp + hh_
                a_ = ar_[pi][hh_]; w_ = ww_[pi][hh_]
                act(w_, a_, AF.Exp)
                if diag:
                    tt("pool", w_, w_, masks[:, B - 4 * c_, :], ALU.mult)
                mm(ob[hh_ * 64:(hh_ + 1) * 64, :], vc[:, B, h * 64:(h + 1) * 64], w_, firstB, lastB)
            if lastB:
                cp("act", bT[:, hp, :], ob)

        s1(0)
        for n in range(len(items)):
            if n + 1 < len(items):
                s1(n + 1)
            part1(n)
            if n >= 1:
                part2(n - 1)
        part2(len(items) - 1)
        A.release(mk)
        if first:
            dump("bT", bT.rearrange("p a b -> p (a b)"), [128, 3 * NCHUNK])
        if l == 0:
            emit_conv(CONV_SLICE)
        mk = A.mark()
        merged = A.alloc([8, NCHUNK], BF16)
        gte = [A.alloc([NCHUNK], F32) for _ in range(2)]
        macc = A.alloc([NCHUNK], F32)
        mtmp = A.alloc([NCHUNK], F32)
        branches = [aT, bT, cT]
        gi = 0
        wbr_t = {}
        win_g = {}
        for dt_ in range(8):
            for n in range(3):
                if dt_ % 2 == 0:
                    wbr_t[n] = wload("wbr", f"wbr{l}_{n}", dt_ // 2, slot=n)
                col = n * D + dt_ * 128
                gtile = 6 + col // 384
                goff = col % 384
                if win_g.get(n, (None, None))[0] != gtile:
                    win_g[n] = (gtile, wload("win", f"win{l}", gtile, slot=n))
                wgt = win_g[n][1]
                pbr = nb()
                for kt in range(3):
                    mm(pbr, wbr_t[n][:, kt, (dt_ % 2) * 128:(dt_ % 2 + 1) * 128], branches[n][:, kt, :], kt == 0, kt == 2)
                pg = nb()
                for dk in range(8):
                    mm(pg, wgt[:, dk, goff:goff + 128], hn[:, dk, :], dk == 0, dk == 7)
                g_ = gte[gi % 2]
                gi += 1
                act(g_, pg, AF.Sigmoid, bias=pcol("gate_b", l, n * 8 + dt_))
                if n == 0:
                    tt("dve", macc, pbr, g_, ALU.mult)
                elif n == 1:
                    tt("dve", mtmp, pbr, g_, ALU.mult)
                    tt("pool", macc, macc, mtmp, ALU.add)
                else:
                    tt("dve", mtmp, pbr, g_, ALU.mult)
                    tt("pool", merged[:, dt_, :], macc, mtmp, ALU.add)
        if first:
            dump("merged", merged.rearrange("p a b -> p (a b)"), [128, 8 * NCHUNK])
        for d2 in range(8):
            if d2 % 2 == 0:
                wo = wload("wout", f"wout{l}", d2 // 2)
            pb = nb()
            for dk in range(8):
                mm(pb, wo[:, dk, (d2 % 2) * 128:(d2 % 2 + 1) * 128], merged[:, dk, :], dk == 0, dk == 7)
            tt("dve", xT[:, d2, :], pb, xT[:, d2, :], ALU.add)
        A.release(mk)
        A.release(mkL)
        if first:
            dump("x1", xT.rearrange("p a b -> p (a b)"), [128, 8 * NCHUNK])

    def ffn_dense(l, xT):
        mkF = A.mark()
        wring("gu", [8, 256], 4)
        wring("wd", [28, 128], 3)
        rmsnorm(xT, "ffn_g", l, out_bf=hn)
        nft = DFF // 128
        actb = A.alloc([nft, NCHUNK], BF16)
        sgb = [A.alloc([NCHUNK], BF16) for _ in range(2)]
        for ft in range(nft):
            if ft % 2 == 0:
                wg_ = wload("gu", "fg", ft // 2)
                wu_ = wload("gu", "fu", ft // 2)
            pg = nb(); pu = nb()
            cs_ = slice((ft % 2) * 128, (ft % 2 + 1) * 128)
            for dk in range(8):
                mm(pg, wg_[:, dk, cs_], hn[:, dk, :], dk == 0, dk == 7)
            for dk in range(8):
                mm(pu, wu_[:, dk, cs_], hn[:, dk, :], dk == 0, dk == 7)
            act(sgb[ft % 2], pg, AF.Silu)
            tt("dve", actb[:, ft, :], pu, sgb[ft % 2], ALU.mult)
        for d2 in range(8):
            wd_ = wload("wd", "fd", d2)
            pb = nb()
            for ft in range(nft):
                mm(pb, wd_[:, ft, :], actb[:, ft, :], ft == 0, ft == nft - 1)
            tt("dve", xT[:, d2, :], pb, xT[:, d2, :], ALU.add)
        A.release(mkF)

    def ffn_moe(l, xT, first):
        mkF = A.mark()
        wring("gu", [8, 256], 4)
        wring("wd", [28, 128], 3)
        combbc = A.alloc([8, NCHUNK], BF16)
        comb = A.alloc([4, 8], F32, align=64)
        mkR = A.mark()
        hnf = A.alloc([8, NCHUNK], F32)
        rmsnorm(xT, "ffn_g", l, out_bf=hn, out_f=hnf)
        lg = A.alloc([4, 8], F32, align=64)
        lg2 = A.alloc([4, 8], F32, align=64)
        eq1 = A.alloc([4, 8], F32, align=64)
        eq2 = A.alloc([4, 8], F32, align=64)
        m1 = A.alloc([4], F32, align=64); m2 = A.alloc([4], F32, align=64)
        dd = A.alloc([4], F32, align=64); w1 = A.alloc([4], F32, align=64); w2 = A.alloc([4], F32, align=64)
        lcol = [A.alloc([128], F32) for _ in range(2)]
        pl = nb()
        for tt_ in range(4):
            for dk in range(8):
                mm(pl[:, tt_ * 8:(tt_ + 1) * 8], hnf[:, dk, tt_ * 128:(tt_ + 1) * 128], rw[:, dk, :], dk == 0, dk == 7)
        tt("dve", lg, pl[:, 0:32].rearrange("p (a b) -> p a b", a=4), rb_bc.unsqueeze(1).to_broadcast([128, 4, 8]), ALU.add)
        P.op("dve", lambda e, m1=m1, lg=lg: e.tensor_reduce(out=m1, in_=lg, axis=mybir.AxisListType.X, op=ALU.max), r=[lg], w=[m1])
        tt("dve", eq1, lg, m1.unsqueeze(2).to_broadcast([128, 4, 8]), ALU.is_equal)
        stt(lg2, eq1, -1e30, lg, ALU.mult, ALU.add)
        P.op("dve", lambda e, m2=m2, lg2=lg2: e.tensor_reduce(out=m2, in_=lg2, axis=mybir.AxisListType.X, op=ALU.max), r=[lg2], w=[m2])
        tt("dve", eq2, lg2, m2.unsqueeze(2).to_broadcast([128, 4, 8]), ALU.is_equal)
        tt("dve", dd, m2, m1, ALU.subtract)
        act(dd, dd, AF.Exp)
        ts("dve", w1, dd, 1.0, ALU.add)
        P.op("dve", lambda e, w1=w1: e.reciprocal(out=w1, in_=w1), r=[w1], w=[w1])
        tt("dve", w2, dd, w1, ALU.mult)
        tt("dve", eq1, eq1, w1.unsqueeze(2).to_broadcast([128, 4, 8]), ALU.mult)
        tt("dve", eq2, eq2, w2.unsqueeze(2).to_broadcast([128, 4, 8]), ALU.mult)
        tt("dve", comb, eq1, eq2, ALU.add)
        if first:
            dump("comb", comb.rearrange("p a b -> p (a b)"), [128, 32])
        ci = 0
        for e_ in range(NE):
            pbc = nb()
            for tt_ in range(4):
                lc_ = lcol[ci % 2]
                ci += 1
                ts("dve", lc_, ones_f, comb[:, tt_, e_:e_ + 1], ALU.mult)
                mm(pbc[:, tt_ * 128:(tt_ + 1) * 128], lc_, ident_f, True, True)
            cp("act", combbc[:, e_, :], pbc)
        A.release(mkR)
        nft = DFE // 128
        actb = A.alloc([nft, NCHUNK], BF16)
        sgb = [A.alloc([NCHUNK], BF16) for _ in range(2)]
        sgc = [A.alloc([NCHUNK], BF16) for _ in range(2)]
        for e_ in range(NE):
            for ft in range(nft):
                if ft % 2 == 0:
                    wg_ = wload("gu", f"mg{e_}", ft // 2)
                    wu_ = wload("gu", f"mu{e_}", ft // 2)
                pg = nb(); pu = nb()
                cs_ = slice((ft % 2) * 128, (ft % 2 + 1) * 128)
                for dk in range(8):
                    mm(pg, wg_[:, dk, cs_], hn[:, dk, :], dk == 0, dk == 7)
                for dk in range(8):
                    mm(pu, wu_[:, dk, cs_], hn[:, dk, :], dk == 0, dk == 7)
                act(sgb[ft % 2], pg, AF.Silu)
                tt("pool", sgc[ft % 2], sgb[ft % 2], combbc[:, e_, :], ALU.mult)
                tt("dve", actb[:, ft, :], pu, sgc[ft % 2], ALU.mult)
            for d2 in range(8):
                wd_ = wload("wd", f"md{e_}", d2)
                pb = nb()
                for ft in range(nft):
                    mm(pb, wd_[:, ft, :], actb[:, ft, :], ft == 0, ft == nft - 1)
                tt("dve", xT[:, d2, :], pb, xT[:, d2, :], ALU.add)
        A.release(mkF)

    it_ = 0
    CONV_SLICE = (len(dsteps) + NSEQ * NCH * 4 - 1) // (NSEQ * NCH * 4)
    for l in range(DEPTH):
        if l == 1:
            emit_conv(len(dsteps), flush=True)
        prep_layer(l)
        for b in range(NSEQ):
            for c_ in range(NCH):
                t0 = c_ * NCHUNK
                first = (b == 0 and c_ == 0 and l == 0)
                xT = xTs[0]
                it_ += 1
                if l == 0:
                    mk0 = A.mark()
                    xs = [A.alloc([D], F32) for _ in range(4)]
                    for tt_ in range(4):
                        dma(xs[tt_], x[b, t0 + tt_ * 128:t0 + (tt_ + 1) * 128, :], sem=f"xs{tt_}")
                    for dk in range(8):
                        pb = nb()
                        for tt_ in range(4):
                            tr(pb[:, tt_ * 128:(tt_ + 1) * 128], xs[tt_][:, dk * 128:(dk + 1) * 128], ident_f)
                        cp("act" if dk % 2 else "dve", xT[:, dk, :], pb)
                    A.release(mk0)
                else:
                    dma(xT, xmid[b, c_], sem=f"xm{it_ % 2}", r=[("xmid", b, c_)])
                mixer(l, b, c_, xT, first)
                if l == 0:
                    emit_conv(CONV_SLICE)
                if l % 2 == 0:
                    ffn_dense(l, xT)
                else:
                    ffn_moe(l, xT, b == 0 and c_ == 0)
                if first:
                    dump("x2", xT.rearrange("p a b -> p (a b)"), [128, 8 * NCHUNK])
                if l < DEPTH - 1:
                    dma(xmid[b, c_], xT, sem=f"xw{it_ % 2}", w=[("xmid", b, c_)])
                else:
                    mk = A.mark()
                    yT = A.alloc([8, NCHUNK], F32)
                    rmsnorm(xT, "fin_g", 0, out_f=yT)
                    ot = [A.alloc([D], F32) for _ in range(4)]
                    for tt_ in range(4):
                        for half in range(2):
                            pb = nb()
                            for q in range(4):
                                dk = half * 4 + q
                                tr(pb[:, q * 128:(q + 1) * 128], yT[:, dk, tt_ * 128:(tt_ + 1) * 128], ident_f)
                            cp("act" if half else "dve", ot[tt_][:, half * 512:(half + 1) * 512], pb)
                        dma(out[b, t0 + tt_ * 128:t0 + (tt_ + 1) * 128, :], ot[tt_], sem=f"ot{tt_}",
                            w=[("out", b, c_, tt_)], final=True)
                    A.release(mk)

    P.emit(st)
    st.close()
    return nc, dbg_out, A.peak


_WNAMES = ["mix_norm_g", "w_in", "gate_b", "s5_lambda_re", "s5_lambda_im", "s5_log_dt", "s5_b_re", "s5_b_im",
           "s5_c_re", "s5_c_im", "s5_d", "s5_w_glu", "s5_b_glu", "conv_w", "conv_b", "lru_w_a", "lru_b_a",
           "lru_w_x", "lru_b_x", "lru_lambda", "w_branch", "w_out", "ffn_norm_g", "ffn_w_gate", "ffn_w_up",
           "ffn_w_down", "router_w", "router_b", "moe_w_gate", "moe_w_up", "moe_w_down", "final_norm_g"]


def kernel(**inputs):
    x = np.ascontiguousarray(np.asarray(inputs["x"], dtype=np.float32))
    ws = {k: np.ascontiguousarray(np.asarray(inputs[k], dtype=np.float32)) for k in _WNAMES}
    nc, _, _ = build()
    in_maps = []
    for i in range(8):
        m = dict(ws)
        m["x"] = x[2 * i:2 * i + 2]
        in_maps.append(m)
    res = run_bass_kernel_spmd(nc, in_maps, core_ids=list(range(8)))
    return np.concatenate([np.asarray(r["out"]) for r in res.results], axis=0).astype(np.float32)
```

```python
import math
import numpy as np
from contextlib import ExitStack
import concourse.bass as bass
import concourse.mybir as mybir
from concourse.bass_utils import run_bass_kernel_spmd

F32 = mybir.dt.float32
BF16 = mybir.dt.bfloat16
U8 = mybir.dt.uint8
AF = mybir.ActivationFunctionType
ALU = mybir.AluOpType

ENGS = ("pe", "act", "dve", "pool", "sp")
EPOCH = 30000
DEFER = False
PRUNE = "pe"
PG = 512

D = 1024
T = 2048
NCHUNK = 512
W = 384
INW = 5376
DFF = 2816
DFE = 3584
NE = 8
LS5 = 128
PI = math.pi


def _dsize(dt):
    return mybir.dt.size(dt)


def keys_of(a):
    if not hasattr(a, "tensor"):
        return [a]
    t = a.tensor
    es = _dsize(a.dtype)
    shp = list(t.shape)
    rb = 1
    for s in shp[1:]:
        rb *= s
    rb *= _dsize(t.dtype)
    ob = a.offset * es
    col0 = ob % rb
    ext = es
    for (step, cnt) in list(a.ap)[1:]:
        ext += (cnt - 1) * abs(step) * es
    p0 = col0 // PG
    p1 = (col0 + ext - 1) // PG
    return [(t.name, p) for p in range(p0, p1 + 1)]


class Prog:
    def __init__(self, nc):
        self.nc = nc
        self.ops = []
        self.last_w = {}
        self.readers = {}
        self.dma_cnt = {}

    def op(self, eng, fn, r=(), w=(), dma=False, sem=None, final=False):
        idx = len(self.ops)
        rk = []
        for a in r:
            rk.extend(keys_of(a))
        wk = []
        for a in w:
            wk.extend(keys_of(a))
        deps = set()
        for k in rk:
            lw = self.last_w.get(k)
            if lw is not None:
                deps.add(lw)
        for k in wk:
            lw = self.last_w.get(k)
            if lw is not None:
                deps.add(lw)
            for rd in self.readers.get(k, ()):
                deps.add(rd)
        deps.discard(idx)
        if eng == "pe":
            deps = {d for d in deps if self.ops[d]["eng"] != "pe"}
        dma_need = {}
        latest = {}
        for d_ in deps:
            od = self.ops[d_]
            if od["dma"]:
                dma_need["d_" + od["sem"]] = 16 * self.dma_cnt[od["sem"]]
            else:
                if latest.get(od["eng"], -1) < d_:
                    latest[od["eng"]] = d_
        if PRUNE == "pe":
            deps = {d_ for d_ in deps if self.ops[d_]["dma"] or self.ops[d_]["eng"] != "pe"}
            if "pe" in latest:
                deps.add(latest["pe"])
        elif PRUNE == "all":
            deps = {d_ for d_ in deps if self.ops[d_]["dma"]} | set(latest.values())
        rec = dict(eng=eng, fn=fn, deps=sorted(deps), dma=dma, sem=None, cnt=None,
                   signal=False, final=final, dma_need=dma_need)
        if dma:
            assert sem is not None
            c = self.dma_cnt.get(sem, 0) + 1
            self.dma_cnt[sem] = c
            rec["sem"] = sem
            rec["cnt"] = c
            rec["signal"] = True
        self.ops.append(rec)
        for k in rk:
            self.readers.setdefault(k, []).append(idx)
        for k in wk:
            self.last_w[k] = idx
            self.readers[k] = []
        return idx

    def emit(self, stack):
        nc = self.nc
        ops = self.ops
        for o in ops:
            for d in o["deps"]:
                ops[d]["signal"] = True
        eng_sig = {e: 0 for e in ENGS}
        for o in ops:
            if o["dma"]:
                continue
            if o["signal"]:
                eng_sig[o["eng"]] += 1
                o["cnt"] = eng_sig[o["eng"]]
        sems = {}

        def getsem(name):
            if name not in sems:
                sems[name] = stack.enter_context(nc.semaphore(name))
            return sems[name]

        for e in ENGS:
            for ep in range(eng_sig[e] // EPOCH + 1):
                getsem(f"e_{e}_{ep}")
        for name in self.dma_cnt:
            getsem("d_" + name)

        def token(o):
            if o["dma"]:
                return ("d_" + o["sem"], 16 * o["cnt"])
            c = o["cnt"]
            ep = (c - 1) // EPOCH
            return (f"e_{o['eng']}_{ep}", c - ep * EPOCH)

        streams = {e: [] for e in ENGS}
        for i, o in enumerate(ops):
            streams[o["eng"]].append(i)
        block = stack.enter_context(nc.Block())

        def run_stream(e, engobj):
            waited = {}
            for i in streams[e]:
                o = ops[i]
                need = {}
                for d in o["deps"]:
                    if ops[d]["dma"]:
                        continue
                    sn, v = token(ops[d])
                    if need.get(sn, 0) < v:
                        need[sn] = v
                for sn, v in o["dma_need"].items():
                    if need.get(sn, 0) < v:
                        need[sn] = v
                for sn, v in need.items():
                    if waited.get(sn, 0) >= v:
                        continue
                    engobj.wait_ge(sems[sn], v)
                    waited[sn] = v
                ins = o["fn"](engobj)
                if o["signal"]:
                    sn, v = token(o)
                    ins.then_inc(sems[sn], 16 if o["dma"] else 1)
            for i in streams[e]:
                o = ops[i]
                if o["dma"] and o["final"]:
                    sn, v = token(o)
                    if waited.get(sn, 0) < v:
                        engobj.wait_ge(sems[sn], v)
                        waited[sn] = v

        @block.tensor
        def _(eng):
            run_stream("pe", eng)

        @block.scalar
        def _(eng):
            run_stream("act", eng)

        @block.vector
        def _(eng):
            run_stream("dve", eng)

        @block.gpsimd
        def _(eng):
            run_stream("pool", eng)

        @block.sync
        def _(eng):
            run_stream("sp", eng)


class Arena:
    def __init__(self, nc, stack, nbytes):
        self.t = stack.enter_context(nc.sbuf_tensor("arena", [128, nbytes], U8))
        self.n = nbytes
        self.top = 0
        self.peak = 0

    def alloc(self, shape, dt=F32, align=PG):
        n = _dsize(dt)
        for s in shape:
            n *= s
        off = (self.top + align - 1) // align * align
        assert off + n <= self.n, f"arena overflow: need {off + n} have {self.n}"
        self.top = off + n
        self.peak = max(self.peak, self.top)
        v = self.t[:, off:off + n].bitcast(dt)
        if len(shape) == 2:
            v = v.rearrange("p (a b) -> p a b", a=shape[0])
        elif len(shape) == 3:
            v = v.rearrange("p (a b c) -> p a b c", a=shape[0], b=shape[1])
        return v

    def mark(self):
        return self.top

    def release(self, m):
        self.top = m


WSPEC = {}


def build(NSEQ=2, NCH=4, DEPTH=2, dbg=(), skip_conv=False):
    nc = bass.Bass("TRN2", target_bir_lowering=False)
    P = Prog(nc)
    st = ExitStack()
    dbg_out = {}

    def din(name, shape):
        return nc.dram_tensor(name, list(shape), F32, kind="ExternalInput").ap()

    x = din("x", [NSEQ, T, D])
    mix_norm_g = din("mix_norm_g", [2, D])
    w_in = din("w_in", [2, D, INW])
    gate_b = din("gate_b", [2, 3 * D])
    s5_lambda_re = din("s5_lambda_re", [2, 24, 64])
    s5_lambda_im = din("s5_lambda_im", [2, 24, 64])
    s5_log_dt = din("s5_log_dt", [2, 24])
    s5_b_re = din("s5_b_re", [2, 24, 64, 16])
    s5_b_im = din("s5_b_im", [2, 24, 64, 16])
    s5_c_re = din("s5_c_re", [2, 24, 16, 64])
    s5_c_im = din("s5_c_im", [2, 24, 16, 64])
    s5_d = din("s5_d", [2, W])
    s5_w_glu = din("s5_w_glu", [2, W, W])
    s5_b_glu = din("s5_b_glu", [2, W])
    conv_w = din("conv_w", [2, 4, W])
    conv_b = din("conv_b", [2, W])
    lru_w_a = din("lru_w_a", [2, 6, 64, 64])
    lru_b_a = din("lru_b_a", [2, W])
    lru_w_x = din("lru_w_x", [2, 6, 64, 64])
    lru_b_x = din("lru_b_x", [2, W])
    lru_lambda = din("lru_lambda", [2, W])
    w_branch = din("w_branch", [2, 3, W, D])
    w_out = din("w_out", [2, D, D])
    ffn_norm_g = din("ffn_norm_g", [2, D])
    ffn_w_gate = din("ffn_w_gate", [1, D, DFF])
    ffn_w_up = din("ffn_w_up", [1, D, DFF])
    ffn_w_down = din("ffn_w_down", [1, DFF, D])
    router_w = din("router_w", [1, D, NE])
    router_b = din("router_b", [1, NE])
    moe_w_gate = din("moe_w_gate", [1, NE, D, DFE])
    moe_w_up = din("moe_w_up", [1, NE, D, DFE])
    moe_w_down = din("moe_w_down", [1, NE, DFE, D])
    final_norm_g = din("final_norm_g", [D])
    out = nc.dram_tensor("out", [NSEQ, T, D], F32, kind="ExternalOutput").ap()

    A = Arena(nc, st, 200704)
    psb = [st.enter_context(nc.psum_tensor(f"ps{i}", [128, 512], F32)).ap() for i in range(8)]
    ps_rr = [0]

    def nb():
        i = ps_rr[0]
        ps_rr[0] = (i + 1) % 8
        return psb[i]

    def dump(name, ap2d, shape):
        if name not in dbg:
            return
        o = nc.dram_tensor("dbg_" + name, list(shape), ap2d.dtype, kind="ExternalOutput").ap()
        dbg_out[name] = o
        P.op("sp", lambda e: e.dma_start(out=o, in_=ap2d), r=[ap2d], w=["dbg_" + name],
             dma=True, sem="dbg_" + name, final=True)

    def act(out_, in_, func, r=None, **kw):
        rr = [in_] + [v for v in kw.values() if hasattr(v, "tensor")]
        P.op("act", lambda e: e.activation(out=out_, in_=in_, func=func, **kw), r=rr, w=[out_])

    def tt(eng, out_, a, b, op):
        P.op(eng, lambda e: e.tensor_tensor(out=out_, in0=a, in1=b, op=op), r=[a, b], w=[out_])

    def ts(eng, out_, a, s1, op0, s2=None, op1=None):
        rr = [a] + [s for s in (s1, s2) if hasattr(s, "tensor")]
        if op1 is None:
            P.op(eng, lambda e: e.tensor_scalar(out=out_, in0=a, scalar1=s1, scalar2=None, op0=op0), r=rr, w=[out_])
        else:
            P.op(eng, lambda e: e.tensor_scalar(out=out_, in0=a, scalar1=s1, scalar2=s2, op0=op0, op1=op1), r=rr, w=[out_])

    def stt(out_, a, s, b, op0, op1):
        rr = [a, b] + ([s] if hasattr(s, "tensor") else [])
        P.op("dve", lambda e: e.scalar_tensor_tensor(out=out_, in0=a, scalar=s, in1=b, op0=op0, op1=op1), r=rr, w=[out_])

    def cp(eng, out_, in_):
        if eng == "act":
            P.op("act", lambda e: e.activation(out=out_, in_=in_, func=AF.Copy), r=[in_], w=[out_])
        else:
            P.op(eng, lambda e: e.tensor_copy(out=out_, in_=in_), r=[in_], w=[out_])

    def memset(eng, ap, val):
        P.op(eng, lambda e: e.memset(ap, val), w=[ap])

    def mm(out_, lhsT, rhs, start, stop):
        P.op("pe", lambda e: e.matmul(out_, lhsT=lhsT, rhs=rhs, start=start, stop=stop), r=[lhsT, rhs], w=[out_])

    def tr(out_, in_, ident):
        P.op("pe", lambda e: e.transpose(out=out_, in_=in_, identity=ident), r=[in_, ident], w=[out_])

    def dma(out_, in_, sem, r=(), w=(), final=False, slow=False):
        rr = list(r) + ([in_] if in_.tensor.name == "arena" else [])
        ww = list(w) + ([out_] if out_.tensor.name == "arena" else [])
        if slow:
            P.op("sp", lambda e: e.dma_start(out=out_, in_=in_, allow_slow_non_contiguous=True), r=rr, w=ww, dma=True, sem=sem, final=final)
        else:
            P.op("sp", lambda e: e.dma_start(out=out_, in_=in_), r=rr, w=ww, dma=True, sem=sem, final=final)

    ident_f = A.alloc([128], F32)
    ones_f = A.alloc([128], F32)
    ident_b = A.alloc([128], BF16, align=256)
    ones_b = A.alloc([128], BF16, align=256)
    trineg = A.alloc([128], BF16, align=256)
    masks = A.alloc([4, 512], BF16)
    tmpc = A.alloc([512], F32)
    memset("pool", ones_f, 1.0)
    P.op("pool", lambda e: e.affine_select(out=ident_f, in_=ones_f, pattern=[[-1, 128]], compare_op=ALU.is_equal,
                                           fill=0.0, base=0, channel_multiplier=1), r=[ones_f], w=[ident_f])
    cp("pool", ident_b, ident_f)
    cp("pool", ones_b, ones_f)
    memset("pool", tmpc, -1.0)
    P.op("pool", lambda e: e.affine_select(out=tmpc[:, 0:128], in_=tmpc[:, 0:128], pattern=[[-1, 128]], compare_op=ALU.is_ge,
                                           fill=0.0, base=0, channel_multiplier=1), r=[tmpc], w=[tmpc])
    cp("pool", trineg, tmpc[:, 0:128])
    for r_ in range(4):
        memset("pool", tmpc, 1.0)
        P.op("pool", lambda e, r_=r_: e.affine_select(out=tmpc, in_=tmpc, pattern=[[1, 512]], compare_op=ALU.is_gt,
                                                     fill=0.0, base=-128 * r_, channel_multiplier=-1), r=[tmpc], w=[tmpc])
        cp("pool", masks[:, r_, :], tmpc)

    rows = {}
    nrow = [0, 0]
    stage = [A.alloc([128], F32), A.alloc([128], F32)]
    memset("dve", stage[0], 0.0)
    memset("dve", stage[1], 0.0)
    ptab = [A.alloc([128], F32), A.alloc([128], F32)]
    pcount = [0]

    def addvec(name, l, src2d, n, si):
        r0 = nrow[si]
        nrow[si] += n
        assert nrow[si] <= 128
        rows[(name, l)] = (si, r0)
        pcount[0] += 1
        dma(stage[si][r0:r0 + n, :], src2d, sem="pstage", r=["in_" + name])

    ldt_s = A.alloc([2, 2], F32, align=64)
    for l in range(2):
        nrow[l] = 12
        rows[("log_dt", l)] = (l, 0)
        dma(ldt_s[0:12, l, :], s5_log_dt[l].rearrange("(m two) -> m two", two=2), sem=f"pldt{l}")
        cp("dve", stage[l][0:12, :].rearrange("m (two q) -> m two q", two=2),
           ldt_s[0:12, l, :].unsqueeze(2).to_broadcast([12, 2, 64]))
    for l in range(2):
        addvec("mix_g", l, mix_norm_g[l].rearrange("(r c) -> r c", c=128), 8, l)
        addvec("ffn_g", l, ffn_norm_g[l].rearrange("(r c) -> r c", c=128), 8, l)
        addvec("gate_b", l, gate_b[l].rearrange("(r c) -> r c", c=128), 24, l)
        addvec("s5_d", l, s5_d[l].rearrange("(r c) -> r c", c=128), 3, l)
        addvec("b_glu", l, s5_b_glu[l].rearrange("(r c) -> r c", c=128), 3, l)
        addvec("conv_w", l, conv_w[l].rearrange("k (r c) -> (k r) c", c=128), 12, l)
        addvec("conv_b", l, conv_b[l].rearrange("(r c) -> r c", c=128), 3, l)
        addvec("b_a", l, lru_b_a[l].rearrange("(r c) -> r c", c=128), 3, l)
        addvec("b_x", l, lru_b_x[l].rearrange("(r c) -> r c", c=128), 3, l)
        addvec("lam", l, lru_lambda[l].rearrange("(r c) -> r c", c=128), 3, l)
        addvec("lam_re", l, s5_lambda_re[l].rearrange("(m two) q -> m (two q)", two=2), 12, l)
        addvec("lam_im", l, s5_lambda_im[l].rearrange("(m two) q -> m (two q)", two=2), 12, l)
    addvec("fin_g", 0, final_norm_g.rearrange("(r c) -> r c", c=128), 8, 0)
    for si in range(2):
        pb = nb()
        tr(pb[:, 0:128], stage[si], ident_f)
        cp("dve", ptab[si], pb[:, 0:128])

    def pcol(name, l, j=0, n=1):
        si, r0 = rows[(name, l)]
        return ptab[si][:, r0 + j:r0 + j + n]

    rw = A.alloc([8, 8], F32, align=64)
    rb_bc = A.alloc([8], F32, align=64)
    dma(rw, router_w[0].rearrange("(kt p) e -> p kt e", p=128), sem="pstage2")
    dma(rb_bc, router_b[0:1, :].to_broadcast([128, 8]), sem="pstage2")

    scr = {}

    def declare_w(name, K, N, cb):
        kt = K // 128
        nt = N // cb
        t = nc.dram_tensor("scr_" + name, [nt, 128, kt, cb], BF16, kind="Internal").ap()
        scr[name] = (t, kt, nt, cb)

    conv_jobs = []
    for l in range(2):
        declare_w(f"win{l}", D, INW, 384)
        conv_jobs.append((f"win{l}", w_in[l], l))
        for n in range(3):
            declare_w(f"wbr{l}_{n}", W, D, 256)
            conv_jobs.append((f"wbr{l}_{n}", w_branch[l, n], l))
        declare_w(f"wout{l}", D, D, 256)
        conv_jobs.append((f"wout{l}", w_out[l], l))
        declare_w(f"glu{l}", W, W, 384)
        conv_jobs.append((f"glu{l}", s5_w_glu[l], l))
    declare_w("fg", D, DFF, 256)
    declare_w("fu", D, DFF, 256)
    declare_w("fd", DFF, D, 128)
    conv_jobs += [("fg", ffn_w_gate[0], 0), ("fu", ffn_w_up[0], 0), ("fd", ffn_w_down[0], 0)]
    for e_ in range(NE):
        declare_w(f"mg{e_}", D, DFE, 256)
        declare_w(f"mu{e_}", D, DFE, 256)
        declare_w(f"md{e_}", DFE, D, 128)
        conv_jobs += [(f"mg{e_}", moe_w_gate[0, e_], 1), (f"mu{e_}", moe_w_up[0, e_], 1), (f"md{e_}", moe_w_down[0, e_], 1)]
    xmid = nc.dram_tensor("xmid", [NSEQ, NCH, 128, 8, NCHUNK], F32, kind="Internal").ap()

    if not skip_conv:
        mk = A.mark()
        NST = 8
        stg = [A.alloc([2048], F32) for _ in range(NST)]
        stb = [A.alloc([2048], BF16) for _ in range(NST)]
        step = 0
        cast_engs = ["dve", "act"]
        pend = []

        def finish(job):
            (i, st_, t, kti, c0, cw, cb, name) = job
            cp(cast_engs[st_ % 2], stb[i][:, 0:cw], stg[i][:, 0:cw])
            t0 = c0 // cb
            ntl = cw // cb
            dma(t[t0:t0 + ntl, :, kti, :].rearrange("t p c -> p t c"),
                stb[i][:, 0:cw].rearrange("p (t c) -> p t c", c=cb), sem=f"stb{i}",
                w=[("scr", name, tt_, kti) for tt_ in range(t0, t0 + ntl)])

        for (name, src, lyr) in conv_jobs:
            if lyr >= DEPTH or (DEFER and lyr >= 1):
                continue
            t, kt, nt, cb = scr[name]
            N = nt * cb
            CW = (2048 // cb) * cb
            for kti in range(kt):
                for c0 in range(0, N, CW):
                    cw = min(CW, N - c0)
                    i = step % NST
                    if len(pend) >= NST - 1:
                        finish(pend.pop(0))
                    dma(stg[i][:, 0:cw], src[kti * 128:(kti + 1) * 128, c0:c0 + cw], sem=f"stg{i}")
                    pend.append((i, step, t, kti, c0, cw, cb, name))
                    step += 1
        while pend:
            finish(pend.pop(0))
        A.release(mk)

    NSTB = 4
    dstg = [A.alloc([1024], F32) for _ in range(NSTB)] if DEFER else []
    dstb = [A.alloc([1024], BF16) for _ in range(NSTB)] if DEFER else []
    dsteps = []
    if DEFER and not skip_conv and DEPTH > 1:
        for (name, src, lyr) in conv_jobs:
            if lyr < 1:
                continue
            t, kt, nt, cb = scr[name]
            N = nt * cb
            CW = (1024 // cb) * cb
            for kti in range(kt):
                for c0 in range(0, N, CW):
                    dsteps.append((name, src, t, kti, c0, min(CW, N - c0), cb))
    dstate = dict(next=0, pend=[])

    def dfinish(job):
        (i, k, name, src, t, kti, c0, cw, cb) = job
        cp("dve" if k % 2 else "act", dstb[i][:, 0:cw], dstg[i][:, 0:cw])
        t0 = c0 // cb
        ntl = cw // cb
        dma(t[t0:t0 + ntl, :, kti, :].rearrange("t p c -> p t c"),
            dstb[i][:, 0:cw].rearrange("p (t c) -> p t c", c=cb), sem=f"dstb{i}",
            w=[("scr", name, tt_, kti) for tt_ in range(t0, t0 + ntl)])

    def emit_conv(nsteps, flush=False):
        for _ in range(nsteps):
            k = dstate["next"]
            if k >= len(dsteps):
                break
            dstate["next"] = k + 1
            (name, src, t, kti, c0, cw, cb) = dsteps[k]
            i = k % NSTB
            if len(dstate["pend"]) >= NSTB - 1:
                dfinish(dstate["pend"].pop(0))
            dma(dstg[i][:, 0:cw], src[kti * 128:(kti + 1) * 128, c0:c0 + cw], sem=f"dstg{i}")
            dstate["pend"].append((i, k, name, src, t, kti, c0, cw, cb))
        if flush:
            while dstate["pend"]:
                dfinish(dstate["pend"].pop(0))

    rings = {}

    def wring(cls, shape, nbuf):
        rings[cls] = dict(bufs=[A.alloc(shape, BF16) for _ in range(nbuf)], i=0)

    def wload(cls, name, ti, slot=None):
        rg = rings[cls]
        if slot is None:
            i = rg["i"]
            rg["i"] = (i + 1) % len(rg["bufs"])
        else:
            i = slot
        buf = rg["bufs"][i]
        t, kt, nt, cb = scr[name]
        dst = buf[:, 0:kt, 0:cb]
        dma(dst, t[ti], sem=f"w_{cls}_{i}", r=[("scr", name, ti, k) for k in range(kt)])
        return dst

    c = {}
    c["Ere"] = A.alloc([12, LS5], BF16)
    c["Eim"] = A.alloc([12, LS5], BF16)
    c["Bre"] = A.alloc([12, 128], BF16)
    c["Bim"] = A.alloc([12, 128], BF16)
    c["Cre"] = A.alloc([12, 128], BF16)
    c["Cimn"] = A.alloc([12, 128], BF16)
    c["Dd"] = A.alloc([3, 128], BF16, align=256)
    c["r"] = A.alloc([12], F32, align=64)
    c["rtab"] = A.alloc([12, LS5], F32)
    c["Wa"] = A.alloc([3, 128], F32)
    c["Wx"] = A.alloc([3, 128], F32)
    c["clru"] = A.alloc([3], F32, align=64)
    c["kT"] = A.alloc([3, T], BF16)
    c["vc"] = A.alloc([16, W], BF16)
    c["slre"] = A.alloc([12], F32, align=64)
    c["slim"] = A.alloc([12], F32, align=64)
    c["hst"] = A.alloc([3], F32, align=64)
    c["halo"] = A.alloc([3, 4], F32, align=64)

    negones = A.alloc([128], F32)
    memset("pool", negones, -1.0)

    xTs = [A.alloc([8, NCHUNK], F32)]
    hn = A.alloc([8, NCHUNK], BF16)
    GELU = AF.Gelu_apprx_tanh

    def prep_layer(l):
        mk = A.mark()
        sm = lambda: A.alloc([12], F32, align=64)
        lr = pcol("lam_re", l, 0, 12)
        li = pcol("lam_im", l, 0, 12)
        ldt = pcol("log_dt", l, 0, 12)
        dt = sm(); lrd = sm(); th = sm(); ea = c["r"]; tq = sm(); th2 = sm()
        sn = sm(); cs = sm(); are = sm(); aim = sm(); den = sm(); rden = sm(); nr = sm()
        cre = sm(); cim = sm(); t1 = sm(); t2 = sm()
        act(dt, ldt, AF.Exp)
        tt("dve", lrd, lr, dt, ALU.mult)
        tt("dve", th, li, dt, ALU.mult)
        act(ea, lrd, AF.Exp)
        for Pp in (64 * PI, 32 * PI, 16 * PI, 8 * PI, 4 * PI, 2 * PI):
            ts("dve", tq, th, Pp - PI, ALU.is_ge, Pp, ALU.mult)
            tt("dve", th, th, tq, ALU.subtract)
        ts("dve", th2, th, PI / 2, ALU.add)
        ts("dve", tq, th2, PI, ALU.is_ge, 2 * PI, ALU.mult)
        tt("dve", th2, th2, tq, ALU.subtract)
        act(sn, th, AF.Sin)
        act(cs, th2, AF.Sin)
        tt("dve", are, ea, cs, ALU.mult)
        tt("dve", aim, ea, sn, ALU.mult)
        tt("dve", t1, lr, lr, ALU.mult)
        tt("dve", t2, li, li, ALU.mult)
        tt("dve", den, t1, t2, ALU.add)
        P.op("dve", lambda e, rden=rden, den=den: e.reciprocal(out=rden, in_=den), r=[den], w=[rden])
        ts("dve", nr, are, -1.0, ALU.add)
        tt("dve", t1, nr, lr, ALU.mult)
        tt("dve", t2, aim, li, ALU.mult)
        tt("dve", t1, t1, t2, ALU.add)
        tt("dve", cre, t1, rden, ALU.mult)
        tt("dve", t1, aim, lr, ALU.mult)
        tt("dve", t2, nr, li, ALU.mult)
        tt("dve", t1, t1, t2, ALU.subtract)
        tt("dve", cim, t1, rden, ALU.mult)
        Ere = A.alloc([12, LS5], F32)
        Eim = A.alloc([12, LS5], F32)
        cp("dve", Ere[:, :, 0:1], cs.unsqueeze(2))
        cp("dve", Eim[:, :, 0:1], sn.unsqueeze(2))
        ta = A.alloc([12, 64], F32)
        tb = A.alloc([12, 64], F32)
        k = 1
        while k < LS5:
            ck = Ere[:, :, k - 1:k].to_broadcast([128, 12, k])
            sk = Eim[:, :, k - 1:k].to_broadcast([128, 12, k])
            tt("dve", ta[:, :, 0:k], Ere[:, :, 0:k], ck, ALU.mult)
            tt("dve", tb[:, :, 0:k], Eim[:, :, 0:k], sk, ALU.mult)
            tt("dve", Ere[:, :, k:2 * k], ta[:, :, 0:k], tb[:, :, 0:k], ALU.subtract)
            tt("dve", ta[:, :, 0:k], Ere[:, :, 0:k], sk, ALU.mult)
            tt("dve", tb[:, :, 0:k], Eim[:, :, 0:k], ck, ALU.mult)
            tt("dve", Eim[:, :, k:2 * k], ta[:, :, 0:k], tb[:, :, 0:k], ALU.add)
            k *= 2
        memset("pool", c["rtab"], 0.0)
        cp("pool", c["rtab"][:, :, 1:LS5], c["r"].unsqueeze(2).to_broadcast([128, 12, LS5 - 1]))
        cp("dve", c["Ere"], Ere)
        cp("dve", c["Eim"], Eim)
        if l == 0:
            dump("Ere", Ere.rearrange("p a b -> p (a b)"), [128, 12 * LS5])
            dump("Eim", Eim.rearrange("p a b -> p (a b)"), [128, 12 * LS5])
            dump("r", c["r"], [128, 12])
            dump("cre", cre, [128, 12])
            dump("cim", cim, [128, 12])
        bre = A.alloc([12, 16], F32)
        bim = A.alloc([12, 16], F32)
        dma(bre, s5_b_re[l].rearrange("(m two) q h -> (two q) m h", two=2), sem="prep")
        dma(bim, s5_b_im[l].rearrange("(m two) q h -> (two q) m h", two=2), sem="prep")
        Bbr = A.alloc([12, 16], F32)
        Bbi = A.alloc([12, 16], F32)
        u1 = A.alloc([12, 16], F32)
        u2 = A.alloc([12, 16], F32)
        creb = cre.unsqueeze(2).to_broadcast([128, 12, 16])
        cimb = cim.unsqueeze(2).to_broadcast([128, 12, 16])
        tt("dve", u1, bre, creb, ALU.mult)
        tt("dve", u2, bim, cimb, ALU.mult)
        tt("dve", Bbr, u1, u2, ALU.subtract)
        tt("dve", u1, bim, creb, ALU.mult)
        tt("dve", u2, bre, cimb, ALU.mult)
        tt("dve", Bbi, u1, u2, ALU.add)
        X = A.alloc([12, 128], F32)
        for (src_, dstname) in ((Bbr, "Bre"), (Bbi, "Bim")):
            memset("pool", X, 0.0)
            for m in range(12):
                for two in range(2):
                    g = 2 * m + two
                    c0 = 16 * (g % 8)
                    cp("pool", X[two * 64:(two + 1) * 64, m, c0:c0 + 16], src_[two * 64:(two + 1) * 64, m, :])
            for m4 in range(3):
                pb = nb()
                for mi in range(4):
                    tr(pb[:, mi * 128:(mi + 1) * 128], X[:, m4 * 4 + mi, :], ident_f)
                cp("dve", c[dstname][:, m4 * 4:(m4 + 1) * 4, :], pb.rearrange("p (a b) -> p a b", a=4))
        for (srcC, dstname, scale) in ((s5_c_re, "Cre", 1.0), (s5_c_im, "Cimn", -1.0)):
            memset("pool", c[dstname], 0.0)
            for yt in range(3):
                ct = X[:, yt, :]
                srcv = srcC[l].rearrange("(y g) h q -> y (g h) q", g=8)[yt]
                dma(ct[:, 0:64], srcv, sem="prep")
                dma(ct[:, 64:128], srcv, sem="prep")
                pb = nb()
                tr(pb[:, 0:128], ct, ident_f)
                for mi in range(4):
                    m = 4 * yt + mi
                    for two in range(2):
                        g = 2 * m + two
                        c0 = 16 * (g % 8)
                        ts("dve", c[dstname][two * 64:(two + 1) * 64, m, c0:c0 + 16],
                           pb[two * 64:(two + 1) * 64, c0:c0 + 16], scale, ALU.mult)
        for yt in range(3):
            ts("dve", c["Dd"][:, yt, :], ident_f, pcol("s5_d", l, yt), ALU.mult)
        memset("pool", c["Wa"], 0.0)
        memset("pool", c["Wx"], 0.0)
        for kt in range(3):
            for half in range(2):
                hsl = slice(half * 64, (half + 1) * 64)
                dma(c["Wa"][hsl, kt, half * 64:(half + 1) * 64], lru_w_a[l, 2 * kt + half], sem="prep")
                dma(c["Wx"][hsl, kt, half * 64:(half + 1) * 64], lru_w_x[l, 2 * kt + half], sem="prep")
        t3 = A.alloc([3], F32, align=64)
        act(t3, pcol("lam", l, 0, 3), AF.Exp, scale=-1.0)
        act(t3, t3, AF.Ln, bias=1.0)
        ts("dve", c["clru"], t3, -8.0, ALU.mult)
        A.release(mk)

    def rmsnorm(xT, gname, l, out_bf=None, out_f=None):
        mk = A.mark()
        sq = [A.alloc([NCHUNK], BF16), A.alloc([NCHUNK], BF16)]
        std = A.alloc([NCHUNK], F32)
        rstd = A.alloc([NCHUNK], F32)
        pb = nb()
        for dk in range(8):
            act(sq[dk % 2], xT[:, dk, :], AF.Square)
            mm(pb, ones_b, sq[dk % 2], dk == 0, dk == 7)
        act(std, pb, AF.Sqrt, scale=1.0 / D, bias=1e-6)
        P.op("dve", lambda e: e.reciprocal(out=rstd, in_=std), r=[std], w=[rstd])
        for dk in range(8):
            if out_bf is not None:
                stt(out_bf[:, dk, :], xT[:, dk, :], pcol(gname, l, dk), rstd, ALU.mult, ALU.mult)
            if out_f is not None:
                stt(out_f[:, dk, :], xT[:, dk, :], pcol(gname, l, dk), rstd, ALU.mult, ALU.mult)
        A.release(mk)

    def mixer(l, b, c_, xT, first):
        t0 = c_ * NCHUNK
        mkL = A.mark()
        wring("win", [8, 384], 3)
        wring("wbr", [3, 256], 3)
        wring("wout", [8, 256], 2)
        wring("glu", [3, 384], 1)
        rmsnorm(xT, "mix_g", l, out_bf=hn)
        if first:
            dump("hn", hn.rearrange("p a b -> p (a b)"), [128, 8 * NCHUNK])
        uT = A.alloc([3, NCHUNK], BF16)
        qT = A.alloc([3, NCHUNK], BF16)
        xl = A.alloc([3, NCHUNK + 4], F32)
        gy = A.alloc([3, NCHUNK], BF16)
        a1 = A.alloc([3, NCHUNK], BF16)
        aT = A.alloc([3, NCHUNK], BF16)
        bT = A.alloc([3, NCHUNK], BF16)
        cT = A.alloc([3, NCHUNK], BF16)
        kT, vc = c["kT"], c["vc"]
        if c_ == 0:
            memset("pool", xl[:, :, 0:3], 0.0)
        else:
            cp("pool", xl[:, :, 0:3], c["halo"][:, :, 0:3])
        for wt in range(6):
            wtile = wload("win", f"win{l}", wt)
            if wt == 3:
                for tt_ in range(4):
                    pb = nb()
                    for dk in range(8):
                        mm(pb[:, 0:W], hn[:, dk, tt_ * 128:(tt_ + 1) * 128], wtile[:, dk, :], dk == 0, dk == 7)
                    cp("act" if tt_ % 2 else "dve", vc[:, 4 * c_ + tt_, :], pb[:, 0:W])
                continue
            for j in range(3):
                pb = nb()
                for dk in range(8):
                    mm(pb, wtile[:, dk, j * 128:(j + 1) * 128], hn[:, dk, :], dk == 0, dk == 7)
                if wt == 0:
                    cp("act", uT[:, j, :], pb)
                elif wt == 1:
                    act(qT[:, j, :], pb, AF.Copy, scale=0.125)
                elif wt == 2:
                    cp("dve", kT[:, j, t0:t0 + NCHUNK], pb)
                elif wt == 4:
                    cp("dve", xl[:, j, 3:3 + NCHUNK], pb)
                elif wt == 5:
                    act(gy[:, j, :], pb, GELU)
        cp("pool", c["halo"][:, :, 0:3], xl[:, :, NCHUNK:NCHUNK + 3])
        if l == 0:
            emit_conv(CONV_SLICE)
        if first:
            dump("uT", uT.rearrange("p a b -> p (a b)"), [128, 3 * NCHUNK])
            dump("qT", qT.rearrange("p a b -> p (a b)"), [128, 3 * NCHUNK])
            dump("vc", vc[:, 0:4, :].rearrange("p a b -> p (a b)"), [128, 4 * W])
        mk = A.mark()
        NSET = 3
        S5T = [[A.alloc([4, LS5], F32) for _ in range(4)] for _ in range(NSET)]
        S5B = [[A.alloc([4, LS5], BF16) for _ in range(2)] for _ in range(NSET)]
        rin = [[A.alloc([4], F32, align=64) for _ in range(2)] for _ in range(NSET)]
        fl = lambda v: v.rearrange("p a b -> p (a b)")
        xc = A.alloc([NCHUNK], F32); av = A.alloc([NCHUNK], F32); ig = A.alloc([NCHUNK], F32)
        om = A.alloc([NCHUNK], F32); hh = A.alloc([NCHUNK], F32)

        s5_R = [psb[3], psb[5]]
        s5_I = [psb[4], psb[6]]
        s5_Y = psb[7]

        def s5_A(it, sc, yt):
            o = sc * LS5
            b1, b2, b3, b4 = S5T[it % NSET]
            rn = rin[it % NSET]
            m0 = 4 * yt
            R = s5_R[it % 2].rearrange("p (a b) -> p a b", a=4)
            I = s5_I[it % 2].rearrange("p (a b) -> p a b", a=4)
            for mi in range(4):
                mm(R[:, mi, :], c["Bre"][:, m0 + mi, :], uT[:, yt, o:o + LS5], True, True)
                mm(I[:, mi, :], c["Bim"][:, m0 + mi, :], uT[:, yt, o:o + LS5], True, True)
            Er4 = c["Ere"][:, m0:m0 + 4, :]
            Ei4 = c["Eim"][:, m0:m0 + 4, :]
            tt("dve", b1, R, Er4, ALU.mult)
            tt("dve", b2, I, Ei4, ALU.mult)
            tt("dve", b3, I, Er4, ALU.mult)
            tt("dve", b4, R, Ei4, ALU.mult)
            tt("dve", b1, b1, b2, ALU.add)
            tt("pool", b3, b3, b4, ALU.subtract)
            if not (c_ == 0 and sc == 0):
                tt("dve", rn[0], c["r"][:, m0:m0 + 4], c["slre"][:, m0:m0 + 4], ALU.mult)
                tt("dve", rn[1], c["r"][:, m0:m0 + 4], c["slim"][:, m0:m0 + 4], ALU.mult)
                tt("dve", b1[:, :, 0:1], b1[:, :, 0:1], rn[0].unsqueeze(2), ALU.add)
                tt("dve", b3[:, :, 0:1], b3[:, :, 0:1], rn[1].unsqueeze(2), ALU.add)

        def s5_B(it, sc, yt):
            b1, b2, b3, b4 = S5T[it % NSET]
            sbf = S5B[it % NSET]
            m0 = 4 * yt
            Er4 = c["Ere"][:, m0:m0 + 4, :]
            Ei4 = c["Eim"][:, m0:m0 + 4, :]
            rt4 = c["rtab"][:, m0:m0 + 4, :]
            for (z_, w_) in ((b2, b1), (b4, b3)):
                P.op("dve", lambda e, z_=z_, w_=w_, rt4=rt4: e.tensor_tensor_scan(
                    out=fl(z_), data0=fl(rt4), data1=fl(w_), initial=0.0, op0=ALU.mult, op1=ALU.add),
                    r=[w_, rt4], w=[z_])
            tt("dve", b1, b2, Er4, ALU.mult)
            tt("pool", b3, b4, Ei4, ALU.mult)
            tt("pool", sbf[0], b1, b3, ALU.subtract)
            tt("dve", c["slre"][:, m0:m0 + 4].unsqueeze(2), b1[:, :, LS5 - 1:LS5], b3[:, :, LS5 - 1:LS5], ALU.subtract)
            tt("pool", b1, b4, Er4, ALU.mult)
            tt("pool", b3, b2, Ei4, ALU.mult)
            tt("pool", sbf[1], b1, b3, ALU.add)
            tt("dve", c["slim"][:, m0:m0 + 4].unsqueeze(2), b1[:, :, LS5 - 1:LS5], b3[:, :, LS5 - 1:LS5], ALU.add)

        def s5_C(it, sc, yt):
            o = sc * LS5
            sbf = S5B[it % NSET]
            m0 = 4 * yt
            q = it % 4
            Y = s5_Y[:, q * LS5:(q + 1) * LS5]
            mm(Y, c["Dd"][:, yt, :], uT[:, yt, o:o + LS5], True, False)
            for mi in range(4):
                mm(Y, c["Cre"][:, m0 + mi, :], sbf[0][:, mi, :], False, False)
                mm(Y, c["Cimn"][:, m0 + mi, :], sbf[1][:, mi, :], False, mi == 3)
            act(a1[:, yt, o:o + LS5], Y, GELU)

        def lru_1(kt):
            ts("dve", xc, xl[:, kt, 0:NCHUNK], pcol("conv_w", l, 0 * 3 + kt), ALU.mult, pcol("conv_b", l, kt), ALU.add)
            for k in range(1, 4):
                stt(xc, xl[:, kt, k:k + NCHUNK], pcol("conv_w", l, k * 3 + kt), xc, ALU.mult, ALU.add)
            mm(psb[1], c["Wa"][:, kt, :], xc, True, True)
            mm(psb[2], c["Wx"][:, kt, :], xc, True, True)

        def lru_2(kt):
            act(av, psb[1], AF.Sigmoid, bias=pcol("b_a", l, kt))
            act(av, av, AF.Exp, scale=c["clru"][:, kt:kt + 1])
            act(ig, psb[2], AF.Sigmoid, bias=pcol("b_x", l, kt))
            tt("pool", om, av, av, ALU.mult)
            ts("pool", om, om, -1.0, ALU.mult, 1.0, ALU.add)
            act(om, om, AF.Sqrt)
            tt("pool", ig, ig, xc, ALU.mult)
            tt("pool", om, om, ig, ALU.mult)

        def lru_3(kt):
            if c_ == 0:
                P.op("dve", lambda e: e.tensor_tensor_scan(
                    out=hh, data0=av, data1=om, initial=0.0, op0=ALU.mult, op1=ALU.add), r=[av, om], w=[hh])
            else:
                ini = c["hst"][:, kt:kt + 1]
                P.op("dve", lambda e, ini=ini: e.tensor_tensor_scan(
                    out=hh, data0=av, data1=om, initial=ini, op0=ALU.mult, op1=ALU.add), r=[av, om, ini], w=[hh])
            cp("dve", c["hst"][:, kt:kt + 1], hh[:, NCHUNK - 1:NCHUNK])
            tt("pool", cT[:, kt, :], hh, gy[:, kt, :], ALU.mult)

        steps = [(sc * 3 + yt, sc, yt) for sc in range(NCHUNK // LS5) for yt in range(3)]
        ns = len(steps)
        for k in range(ns + 2):
            if k < ns:
                s5_A(*steps[k])
            if 0 <= k - 1 < ns:
                s5_B(*steps[k - 1])
            if 0 <= k - 2 < ns:
                s5_C(*steps[k - 2])
            for kt in range(3):
                r0 = 1 + 4 * kt
                if k == r0:
                    lru_1(kt)
                elif k == r0 + 1:
                    lru_2(kt)
                elif k == r0 + 2:
                    lru_3(kt)
        if l == 0 and DEFER:
            emit_conv(CONV_SLICE)
        wg = wload("glu", f"glu{l}", 0)
        sg = [A.alloc([NCHUNK], BF16), A.alloc([NCHUNK], BF16)]
        for j in range(3):
            pb = nb()
            for kt in range(3):
                mm(pb, wg[:, kt, j * 128:(j + 1) * 128], a1[:, kt, :], kt == 0, kt == 2)
            act(sg[j % 2], pb, AF.Sigmoid, bias=pcol("b_glu", l, j))
            tt("pool", aT[:, j, :], a1[:, j, :], sg[j % 2], ALU.mult)
        A.release(mk)
        if first:
            dump("a1", a1.rearrange("p a b -> p (a b)"), [128, 3 * NCHUNK])
            dump("aT", aT.rearrange("p a b -> p (a b)"), [128, 3 * NCHUNK])
            dump("cT", cT.rearrange("p a b -> p (a b)"), [128, 3 * NCHUNK])
        mk = A.mark()
        NPIPE = 2
        ex_ = [[A.alloc([NCHUNK], BF16) for _ in range(2)] for _ in range(NPIPE)]
        sp_ = ex_
        ar_ = [[A.alloc([NCHUNK], F32) for _ in range(2)] for _ in range(NPIPE)]
        ww_ = [[A.alloc([NCHUNK], BF16) for _ in range(2)] for _ in range(NPIPE)]
        carry = [A.alloc([NCHUNK], F32) for _ in range(2)]
        zb = [[psb[0], psb[1]], [psb[2], psb[3]]]
        ob = psb[4]
        csb = [psb[5], psb[6]]
        items = []
        for hp in range(3):
            for B in range(4 * c_ + 3, -1, -1):
                items.append((hp, B))

        def s1(n):
            hp, B = items[n]
            for hh_ in range(2):
                ps_ = slice(hh_ * 64, (hh_ + 1) * 64)
                mm(zb[n % 2][hh_], kT[ps_, hp, B * 128:(B + 1) * 128], qT[ps_, hp, :], True, False)

        def part1(n):
            hp, B = items[n]
            pi = n % NPIPE
            diag = B >= 4 * c_
            lastB = (B == 0)
            firstB = (B == 4 * c_ + 3)
            for hh_ in range(2):
                z = zb[n % 2][hh_]
                e_ = ex_[pi][hh_]; s_ = sp_[pi][hh_]; a_ = ar_[pi][hh_]
                act(e_, z, AF.Exp)
                act(s_, e_, AF.Ln, bias=1.0)
                if diag:
                    tt("pool", s_, s_, masks[:, B - 4 * c_, :], ALU.mult)
                mm(z, trineg, s_, False, True)
                if firstB:
                    cp("dve", a_, z)
                else:
                    tt("dve", a_, z, carry[hh_], ALU.subtract)
                if not lastB:
                    mm(csb[hh_], ones_b, s_, True, True)
                    if firstB:
                        cp("dve", carry[hh_], csb[hh_])
                    else:
                        tt("dve", carry[hh_], csb[hh_], carry[hh_], ALU.add)

        def part2(n):
            hp, B = items[n]
            pi = n % NPIPE
            diag = B >= 4 * c_
            lastB = (B == 0)
            firstB = (B == 4 * c_ + 3)
            for hh_ in range(2):
                h = 2 * hp + hh_
                a_ = ar_[pi][hh_]; w_ = ww_[pi][hh_]
                act(w_, a_, AF.Exp)
                if diag:
                    tt("pool", w_, w_, masks[:, B - 4 * c_, :], ALU.mult)
                mm(ob[hh_ * 64:(hh_ + 1) * 64, :], vc[:, B, h * 64:(h + 1) * 64], w_, firstB, lastB)
            if lastB:
                cp("act", bT[:, hp, :], ob)

        s1(0)
        for n in range(len(items)):
            if n + 1 < len(items):
                s1(n + 1)
            part1(n)
            if n >= 1:
                part2(n - 1)
        part2(len(items) - 1)
        A.release(mk)
        if first:
            dump("bT", bT.rearrange("p a b -> p (a b)"), [128, 3 * NCHUNK])
        if l == 0:
            emit_conv(CONV_SLICE)
        mk = A.mark()
        merged = A.alloc([8, NCHUNK], BF16)
        gte = [A.alloc([NCHUNK], F32) for _ in range(2)]
        macc = A.alloc([NCHUNK], F32)
        mtmp = A.alloc([NCHUNK], F32)
        branches = [aT, bT, cT]
        gi = 0
        wbr_t = {}
        win_g = {}
        for dt_ in range(8):
            for n in range(3):
                if dt_ % 2 == 0:
                    wbr_t[n] = wload("wbr", f"wbr{l}_{n}", dt_ // 2, slot=n)
                col = n * D + dt_ * 128
                gtile = 6 + col // 384
                goff = col % 384
                if win_g.get(n, (None, None))[0] != gtile:
                    win_g[n] = (gtile, wload("win", f"win{l}", gtile, slot=n))
                wgt = win_g[n][1]
                pbr = nb()
                for kt in range(3):
                    mm(pbr, wbr_t[n][:, kt, (dt_ % 2) * 128:(dt_ % 2 + 1) * 128], branches[n][:, kt, :], kt == 0, kt == 2)
                pg = nb()
                for dk in range(8):
                    mm(pg, wgt[:, dk, goff:goff + 128], hn[:, dk, :], dk == 0, dk == 7)
                g_ = gte[gi % 2]
                gi += 1
                act(g_, pg, AF.Sigmoid, bias=pcol("gate_b", l, n * 8 + dt_))
                if n == 0:
                    tt("dve", macc, pbr, g_, ALU.mult)
                elif n == 1:
                    tt("dve", mtmp, pbr, g_, ALU.mult)
                    tt("pool", macc, macc, mtmp, ALU.add)
                else:
                    tt("dve", mtmp, pbr, g_, ALU.mult)
                    tt("pool", merged[:, dt_, :], macc, mtmp, ALU.add)
        if first:
            dump("merged", merged.rearrange("p a b -> p (a b)"), [128, 8 * NCHUNK])
        for d2 in range(8):
            if d2 % 2 == 0:
                wo = wload("wout", f"wout{l}", d2 // 2)
            pb = nb()
            for dk in range(8):
                mm(pb, wo[:, dk, (d2 % 2) * 128:(d2 % 2 + 1) * 128], merged[:, dk, :], dk == 0, dk == 7)
            tt("dve", xT[:, d2, :], pb, xT[:, d2, :], ALU.add)
        A.release(mk)
        A.release(mkL)
        if first:
            dump("x1", xT.rearrange("p a b -> p (a b)"), [128, 8 * NCHUNK])

    def ffn_dense(l, xT):
        mkF = A.mark()
        wring("gu", [8, 256], 4)
        wring("wd", [28, 128], 3)
        rmsnorm(xT, "ffn_g", l, out_bf=hn)
        nft = DFF // 128
        actb = A.alloc([nft, NCHUNK], BF16)
        sgb = [A.alloc([NCHUNK], BF16) for _ in range(2)]
        for ft in range(nft):
            if ft % 2 == 0:
                wg_ = wload("gu", "fg", ft // 2)
                wu_ = wload("gu", "fu", ft // 2)
            pg = nb(); pu = nb()
            cs_ = slice((ft % 2) * 128, (ft % 2 + 1) * 128)
            for dk in range(8):
                mm(pg, wg_[:, dk, cs_], hn[:, dk, :], dk == 0, dk == 7)
            for dk in range(8):
                mm(pu, wu_[:, dk, cs_], hn[:, dk, :], dk == 0, dk == 7)
            act(sgb[ft % 2], pg, AF.Silu)
            tt("dve", actb[:, ft, :], pu, sgb[ft % 2], ALU.mult)
        for d2 in range(8):
            wd_ = wload("wd", "fd", d2)
            pb = nb()
            for ft in range(nft):
                mm(pb, wd_[:, ft, :], actb[:, ft, :], ft == 0, ft == nft - 1)
            tt("dve", xT[:, d2, :], pb, xT[:, d2, :], ALU.add)
        A.release(mkF)

    def ffn_moe(l, xT, first):
        mkF = A.mark()
        wring("gu", [8, 256], 4)
        wring("wd", [28, 128], 3)
        combbc = A.alloc([8, NCHUNK], BF16)
        comb = A.alloc([4, 8], F32, align=64)
        mkR = A.mark()
        hnf = A.alloc([8, NCHUNK], F32)
        rmsnorm(xT, "ffn_g", l, out_bf=hn, out_f=hnf)
        lg = A.alloc([4, 8], F32, align=64)
        lg2 = A.alloc([4, 8], F32, align=64)
        eq1 = A.alloc([4, 8], F32, align=64)
        eq2 = A.alloc([4, 8], F32, align=64)
        m1 = A.alloc([4], F32, align=64); m2 = A.alloc([4], F32, align=64)
        dd = A.alloc([4], F32, align=64); w1 = A.alloc([4], F32, align=64); w2 = A.alloc([4], F32, align=64)
        lcol = [A.alloc([128], F32) for _ in range(2)]
        pl = nb()
        for tt_ in range(4):
            for dk in range(8):
                mm(pl[:, tt_ * 8:(tt_ + 1) * 8], hnf[:, dk, tt_ * 128:(tt_ + 1) * 128], rw[:, dk, :], dk == 0, dk == 7)
        tt("dve", lg, pl[:, 0:32].rearrange("p (a b) -> p a b", a=4), rb_bc.unsqueeze(1).to_broadcast([128, 4, 8]), ALU.add)
        P.op("dve", lambda e, m1=m1, lg=lg: e.tensor_reduce(out=m1, in_=lg, axis=mybir.AxisListType.X, op=ALU.max), r=[lg], w=[m1])
        tt("dve", eq1, lg, m1.unsqueeze(2).to_broadcast([128, 4, 8]), ALU.is_equal)
        stt(lg2, eq1, -1e30, lg, ALU.mult, ALU.add)
        P.op("dve", lambda e, m2=m2, lg2=lg2: e.tensor_reduce(out=m2, in_=lg2, axis=mybir.AxisListType.X, op=ALU.max), r=[lg2], w=[m2])
        tt("dve", eq2, lg2, m2.unsqueeze(2).to_broadcast([128, 4, 8]), ALU.is_equal)
        tt("dve", dd, m2, m1, ALU.subtract)
        act(dd, dd, AF.Exp)
        ts("dve", w1, dd, 1.0, ALU.add)
        P.op("dve", lambda e, w1=w1: e.reciprocal(out=w1, in_=w1), r=[w1], w=[w1])
        tt("dve", w2, dd, w1, ALU.mult)
        tt("dve", eq1, eq1, w1.unsqueeze(2).to_broadcast([128, 4, 8]), ALU.mult)
        tt("dve", eq2, eq2, w2.unsqueeze(2).to_broadcast([128, 4, 8]), ALU.mult)
        tt("dve", comb, eq1, eq2, ALU.add)
        if first:
            dump("comb", comb.rearrange("p a b -> p (a b)"), [128, 32])
        ci = 0
        for e_ in range(NE):
            pbc = nb()
            for tt_ in range(4):
                lc_ = lcol[ci % 2]
                ci += 1
                ts("dve", lc_, ones_f, comb[:, tt_, e_:e_ + 1], ALU.mult)
                mm(pbc[:, tt_ * 128:(tt_ + 1) * 128], lc_, ident_f, True, True)
            cp("act", combbc[:, e_, :], pbc)
        A.release(mkR)
        nft = DFE // 128
        actb = A.alloc([nft, NCHUNK], BF16)
        sgb = [A.alloc([NCHUNK], BF16) for _ in range(2)]
        sgc = [A.alloc([NCHUNK], BF16) for _ in range(2)]
        for e_ in range(NE):
            for ft in range(nft):
                if ft % 2 == 0:
                    wg_ = wload("gu", f"mg{e_}", ft // 2)
                    wu_ = wload("gu", f"mu{e_}", ft // 2)
                pg = nb(); pu = nb()
                cs_ = slice((ft % 2) * 128, (ft % 2 + 1) * 128)
                for dk in range(8):
                    mm(pg, wg_[:, dk, cs_], hn[:, dk, :], dk == 0, dk == 7)
                for dk in range(8):
                    mm(pu, wu_[:, dk, cs_], hn[:, dk, :], dk == 0, dk == 7)
                act(sgb[ft % 2], pg, AF.Silu)
                tt("pool", sgc[ft % 2], sgb[ft % 2], combbc[:, e_, :], ALU.mult)
                tt("dve", actb[:, ft, :], pu, sgc[ft % 2], ALU.mult)
            for d2 in range(8):
                wd_ = wload("wd", f"md{e_}", d2)
                pb = nb()
                for ft in range(nft):
                    mm(pb, wd_[:, ft, :], actb[:, ft, :], ft == 0, ft == nft - 1)
                tt("dve", xT[:, d2, :], pb, xT[:, d2, :], ALU.add)
        A.release(mkF)

    it_ = 0
    CONV_SLICE = (len(dsteps) + NSEQ * NCH * 4 - 1) // (NSEQ * NCH * 4)
    for l in range(DEPTH):
        if l == 1:
            emit_conv(len(dsteps), flush=True)
        prep_layer(l)
        for b in range(NSEQ):
            for c_ in range(NCH):
                t0 = c_ * NCHUNK
                first = (b == 0 and c_ == 0 and l == 0)
                xT = xTs[0]
                it_ += 1
                if l == 0:
                    mk0 = A.mark()
                    xs = [A.alloc([D], F32) for _ in range(4)]
                    for tt_ in range(4):
                        dma(xs[tt_], x[b, t0 + tt_ * 128:t0 + (tt_ + 1) * 128, :], sem=f"xs{tt_}")
                    for dk in range(8):
                        pb = nb()
                        for tt_ in range(4):
                            tr(pb[:, tt_ * 128:(tt_ + 1) * 128], xs[tt_][:, dk * 128:(dk + 1) * 128], ident_f)
                        cp("act" if dk % 2 else "dve", xT[:, dk, :], pb)
                    A.release(mk0)
                else:
                    dma(xT, xmid[b, c_], sem=f"xm{it_ % 2}", r=[("xmid", b, c_)])
                mixer(l, b, c_, xT, first)
                if l == 0:
                    emit_conv(CONV_SLICE)
                if l % 2 == 0:
                    ffn_dense(l, xT)
                else:
                    ffn_moe(l, xT, b == 0 and c_ == 0)
                if first:
                    dump("x2", xT.rearrange("p a b -> p (a b)"), [128, 8 * NCHUNK])
                if l < DEPTH - 1:
                    dma(xmid[b, c_], xT, sem=f"xw{it_ % 2}", w=[("xmid", b, c_)])
                else:
                    mk = A.mark()
                    yT = A.alloc([8, NCHUNK], F32)
                    rmsnorm(xT, "fin_g", 0, out_f=yT)
                    ot = [A.alloc([D], F32) for _ in range(4)]
                    for tt_ in range(4):
                        for half in range(2):
                            pb = nb()
                            for q in range(4):
                                dk = half * 4 + q
                                tr(pb[:, q * 128:(q + 1) * 128], yT[:, dk, tt_ * 128:(tt_ + 1) * 128], ident_f)
                            cp("act" if half else "dve", ot[tt_][:, half * 512:(half + 1) * 512], pb)
                        dma(out[b, t0 + tt_ * 128:t0 + (tt_ + 1) * 128, :], ot[tt_], sem=f"ot{tt_}",
                            w=[("out", b, c_, tt_)], final=True)
                    A.release(mk)

    P.emit(st)
    st.close()
    return nc, dbg_out, A.peak


_WNAMES = ["mix_norm_g", "w_in", "gate_b", "s5_lambda_re", "s5_lambda_im", "s5_log_dt", "s5_b_re", "s5_b_im",
           "s5_c_re", "s5_c_im", "s5_d", "s5_w_glu", "s5_b_glu", "conv_w", "conv_b", "lru_w_a", "lru_b_a",
           "lru_w_x", "lru_b_x", "lru_lambda", "w_branch", "w_out", "ffn_norm_g", "ffn_w_gate", "ffn_w_up",
           "ffn_w_down", "router_w", "router_b", "moe_w_gate", "moe_w_up", "moe_w_down", "final_norm_g"]


def kernel(**inputs):
    x = np.ascontiguousarray(np.asarray(inputs["x"], dtype=np.float32))
    ws = {k: np.ascontiguousarray(np.asarray(inputs[k], dtype=np.float32)) for k in _WNAMES}
    nc, _, _ = build()
    in_maps = []
    for i in range(8):
        m = dict(ws)
        m["x"] = x[2 * i:2 * i + 2]
        in_maps.append(m)
    res = run_bass_kernel_spmd(nc, in_maps, core_ids=list(range(8)))
    return np.concatenate([np.asarray(r["out"]) for r in res.results], axis=0).astype(np.float32)
```
